# Optimizing a Trainium2 kernel written in Bass

```python
import jax, jax.numpy as jnp
from jax import lax
import numpy as np

D_MODEL = 1024
BATCH = 8
SEQ = 8192
DEPTH = 2

N_BRANCH = 4
BRANCH_WIDTH = D_MODEL // 4
HEAD_DIM = 64
N_HEADS = BRANCH_WIDTH // HEAD_DIM
CONV_WIDTH = 31
ROPE_THETA = 500000.0
ROPE_DIMS = HEAD_DIM // 4
TOPK_MAX = 256
Q_BLOCK = 128
IDX_HEADS = 4
IDX_DIM = 64
GLA_DK = HEAD_DIM // 2
GLA_QK_WIDTH = N_HEADS * GLA_DK
GLA_RANK = 16
GLA_TAU = 16.0
CHUNK = 64
D_FF = 4 * D_MODEL
EPS = 1e-6
POS_OFFSET_MAX = 1024

A_COLS = 2 * BRANCH_WIDTH
B_COLS = BRANCH_WIDTH + 2 * HEAD_DIM + IDX_HEADS * IDX_DIM + IDX_DIM + IDX_HEADS
C_COLS = 2 * GLA_QK_WIDTH + 2 * BRANCH_WIDTH + GLA_RANK
D_COLS = 4 * BRANCH_WIDTH
G_COLS = N_BRANCH * D_MODEL
N_IN = A_COLS + B_COLS + C_COLS + D_COLS + G_COLS

kernel_name = 'hybrid_gated_four_mixer_block'


def _split(z, sizes):
    offs = np.cumsum(sizes)[:-1].tolist()
    return jnp.split(z, offs, axis=-1)


def _rmsnorm(x, g):
    xf = x.astype(jnp.float32)
    y = xf * lax.rsqrt(jnp.mean(xf * xf, axis=-1, keepdims=True) + EPS)
    return (y * g.astype(jnp.float32)).astype(x.dtype)


def _layernorm(x, g, b):
    xf = x.astype(jnp.float32)
    mu = jnp.mean(xf, axis=-1, keepdims=True)
    xc = xf - mu
    var = jnp.mean(xc * xc, axis=-1, keepdims=True)
    return (xc * lax.rsqrt(var + EPS) * g.astype(jnp.float32) + b.astype(jnp.float32)).astype(x.dtype)


def _rope(x, pos):
    half = ROPE_DIMS // 2
    inv = jnp.power(jnp.float32(ROPE_THETA), -jnp.arange(half, dtype=jnp.float32) * (2.0 / ROPE_DIMS))
    ang = pos.astype(jnp.float32)[..., None] * inv
    cos = jnp.cos(ang)[:, :, None, :]
    sin = jnp.sin(ang)[:, :, None, :]
    x1 = x[..., :half].astype(jnp.float32)
    x2 = x[..., half:ROPE_DIMS].astype(jnp.float32)
    rot = jnp.concatenate([x1 * cos - x2 * sin, x1 * sin + x2 * cos], axis=-1).astype(x.dtype)
    return jnp.concatenate([rot, x[..., ROPE_DIMS:]], axis=-1)


def _conv_branch(za, conv_w, conv_b, ln_g, ln_b):
    val, gate = jnp.split(za, 2, axis=-1)
    u = val * jax.nn.sigmoid(gate)
    u = jnp.pad(u, ((0, 0), (CONV_WIDTH - 1, 0), (0, 0)))
    u = lax.conv_general_dilated(u, conv_w[:, None, :].astype(u.dtype), window_strides=(1,),
                                 padding='VALID', dimension_numbers=('NWC', 'WIO', 'NWC'),
                                 feature_group_count=BRANCH_WIDTH) + conv_b.astype(u.dtype)
    return jax.nn.silu(_layernorm(u, ln_g, ln_b))


def _sparse_attn_branch(zb, pos):
    B_, S_ = zb.shape[0], zb.shape[1]
    q, k, v, qi, ki, wi = _split(zb, [BRANCH_WIDTH, HEAD_DIM, HEAD_DIM, IDX_HEADS * IDX_DIM, IDX_DIM, IDX_HEADS])
    q = _rope(q.reshape(B_, S_, N_HEADS, HEAD_DIM), pos)
    k = _rope(k.reshape(B_, S_, 1, HEAD_DIM), pos)[:, :, 0]
    qi = _rope(qi.reshape(B_, S_, IDX_HEADS, IDX_DIM), pos)
    ki = _rope(ki.reshape(B_, S_, 1, IDX_DIM), pos)[:, :, 0]
    wi = wi * (IDX_HEADS ** -0.5)
    topk = min(TOPK_MAX, S_ // 4)
    nb = S_ // Q_BLOCK
    key_pos = jnp.arange(S_)

    def blockify(a):
        return a.reshape((B_, nb, Q_BLOCK) + a.shape[2:]).swapaxes(0, 1)

    def one_block(args):
        qb, qib, wib, blk = args
        tq = blk * Q_BLOCK + jnp.arange(Q_BLOCK)
        causal = key_pos[None, :] <= tq[:, None]
        logits = jnp.einsum('bqhd,bsd->bqhs', qib, ki) * (IDX_DIM ** -0.5)
        score = jnp.einsum('bqh,bqhs->bqs', wib, jax.nn.relu(logits)).astype(jnp.float32)
        score = jnp.where(causal[None], score, -jnp.inf)
        _, idx = lax.top_k(score, topk)
        valid = idx <= tq[None, :, None]
        ks = jax.vmap(lambda kk, ii: kk[ii])(k, idx)
        vs = jax.vmap(lambda vv, ii: vv[ii])(v, idx)
        s = jnp.einsum('bqhd,bqkd->bhqk', qb, ks).astype(jnp.float32) * (HEAD_DIM ** -0.5)
        s = jnp.where(valid[:, None], s, -jnp.inf)
        p = jax.nn.softmax(s, axis=-1).astype(vs.dtype)
        return jnp.einsum('bhqk,bqkd->bqhd', p, vs)

    out = lax.map(one_block, (blockify(q), blockify(qi), blockify(wi), jnp.arange(nb)))
    return out.swapaxes(0, 1).reshape(B_, S_, BRANCH_WIDTH)


def _chunked_gated_linear_attn(q, k, v, log_f):
    B_, S_, H_, dk = q.shape
    dv = v.shape[-1]
    nc = S_ // CHUNK

    def chunks(a):
        return a.astype(jnp.float32).reshape(B_, nc, CHUNK, H_, a.shape[-1]).transpose(1, 0, 3, 2, 4)

    qc, kc, vc = chunks(q), chunks(k), chunks(v)
    gc = jnp.cumsum(chunks(log_f), axis=3)
    causal = jnp.tril(jnp.ones((CHUNK, CHUNK), dtype=bool))

    def step(state, inp):
        qb, kb, vb, gb = inp
        diff = gb[:, :, :, None, :] - gb[:, :, None, :, :]
        decay = jnp.exp(jnp.where(causal[:, :, None], diff, -jnp.inf))
        attn = jnp.einsum('bhid,bhjd,bhijd->bhij', qb, kb, decay)
        o = jnp.einsum('bhij,bhjv->bhiv', attn, vb) + jnp.einsum('bhid,bhdv->bhiv', qb * jnp.exp(gb), state)
        g_last = gb[:, :, -1:, :]
        state = state * jnp.exp(g_last[:, :, 0, :, None]) + jnp.einsum('bhjd,bhjv->bhdv', kb * jnp.exp(g_last - gb), vb)
        return state, o

    state0 = jnp.zeros((B_, H_, dk, dv), jnp.float32)
    _, o = lax.scan(step, state0, (qc, kc, vc, gc))
    return o.transpose(1, 0, 3, 2, 4).reshape(B_, S_, H_, dv).astype(v.dtype)


def _gla_branch(zc, gate_w, gate_b, norm_g):
    B_, S_ = zc.shape[0], zc.shape[1]
    q, k, v, og, glr = _split(zc, [GLA_QK_WIDTH, GLA_QK_WIDTH, BRANCH_WIDTH, BRANCH_WIDTH, GLA_RANK])
    q = q.reshape(B_, S_, N_HEADS, GLA_DK) * (GLA_DK ** -0.5)
    k = k.reshape(B_, S_, N_HEADS, GLA_DK)
    v = v.reshape(B_, S_, N_HEADS, HEAD_DIM)
    log_a = jax.nn.log_sigmoid((glr @ gate_w + gate_b).astype(jnp.float32)) / GLA_TAU
    o = _chunked_gated_linear_attn(q, k, v, log_a.reshape(B_, S_, N_HEADS, GLA_DK))
    return _rmsnorm(o, norm_g).reshape(B_, S_, BRANCH_WIDTH) * jax.nn.silu(og)


def _hgrn2_branch(zd, lb, norm_g):
    B_, S_ = zd.shape[0], zd.shape[1]
    f_in, q, i, og = _split(zd, [BRANCH_WIDTH] * 4)
    zf = f_in.astype(jnp.float32)
    f = lb + (1.0 - lb) * jax.nn.sigmoid(zf)
    log_f = jnp.log(f)
    k = (1.0 - lb) * jax.nn.sigmoid(-zf)
    q = jax.nn.silu(q)
    shp = (B_, S_, N_HEADS, HEAD_DIM)
    o = _chunked_gated_linear_attn(q.reshape(shp), k.reshape(shp), i.reshape(shp), log_f.reshape(shp))
    return _rmsnorm(o, norm_g).reshape(B_, S_, BRANCH_WIDTH) * jax.nn.silu(og)


def setup_inputs(seed: int = 0) -> dict:
    key = jax.random.key(seed)
    ks = jax.random.split(key, 24)
    L, D, W = DEPTH, D_MODEL, BRANCH_WIDTH

    def nrm(k, shape, fan_in):
        return jax.random.normal(k, shape, jnp.float32) * (fan_in ** -0.5)

    def small(k, shape, s):
        return jax.random.normal(k, shape, jnp.float32) * s

    x = jax.random.normal(ks[0], (BATCH, SEQ, D), jnp.float32)
    c = jax.random.normal(ks[1], (BATCH, D), jnp.float32)
    positions = jnp.arange(SEQ, dtype=jnp.int32)[None, :] + jax.random.randint(ks[2], (BATCH, 1), 0, POS_OFFSET_MAX, dtype=jnp.int32)
    return {
        'x': x,
        'c': c,
        'positions': positions,
        'ada_w': nrm(ks[3], (L, D, 6 * D), D) * 0.5,
        'ada_b': small(ks[4], (L, 6 * D), 0.02),
        'norm_mix_g': 1.0 + small(ks[5], (L, D), 0.02),
        'norm_mlp_g': 1.0 + small(ks[6], (L, D), 0.02),
        'w_in': nrm(ks[7], (L, D, N_IN), D),
        'conv_w': nrm(ks[8], (L, CONV_WIDTH, W), CONV_WIDTH),
        'conv_b': small(ks[9], (L, W), 0.02),
        'conv_ln_g': 1.0 + small(ks[10], (L, W), 0.02),
        'conv_ln_b': small(ks[11], (L, W), 0.02),
        'gla_gate_w': nrm(ks[12], (L, GLA_RANK, GLA_QK_WIDTH), GLA_RANK),
        'gla_gate_b': small(ks[13], (L, GLA_QK_WIDTH), 0.1),
        'gla_norm_g': 1.0 + small(ks[14], (L, HEAD_DIM), 0.02),
        'hgrn_lb_logits': small(ks[15], (L, W), 0.1),
        'hgrn_norm_g': 1.0 + small(ks[16], (L, HEAD_DIM), 0.02),
        'w_branch_out': nrm(ks[17], (L, N_BRANCH, W, D), W),
        'w_o': nrm(ks[18], (L, D, D), D),
        'mlp_w1': nrm(ks[19], (L, D, D_FF), D),
        'mlp_w2': nrm(ks[20], (L, D_FF, D), D_FF),
        'final_g': 1.0 + small(ks[21], (D,), 0.02),
    }


def reference(x, c, positions, ada_w, ada_b, norm_mix_g, norm_mlp_g, w_in, conv_w, conv_b, conv_ln_g,
              conv_ln_b, gla_gate_w, gla_gate_b, gla_norm_g, hgrn_lb_logits, hgrn_norm_g, w_branch_out,
              w_o, mlp_w1, mlp_w2, final_g):
    B_, S_, D_ = x.shape
    p_lb = jax.nn.softmax(hgrn_lb_logits.astype(jnp.float32), axis=0)
    lower_bounds = jnp.cumsum(p_lb, axis=0) - p_lb[0:1]
    c_act = jax.nn.silu(c)
    for l in range(DEPTH):
        cmod = (c_act @ ada_w[l] + ada_b[l])[:, None, :]
        sh1, sc1, g1, sh2, sc2, g2 = jnp.split(cmod, 6, axis=-1)
        h = _rmsnorm(x, norm_mix_g[l]) * (1.0 + sc1) + sh1
        z = h @ w_in[l]
        za, zb, zc, zd, zg = _split(z, [A_COLS, B_COLS, C_COLS, D_COLS, G_COLS])
        ya = _conv_branch(za, conv_w[l], conv_b[l], conv_ln_g[l], conv_ln_b[l])
        yb = _sparse_attn_branch(zb, positions)
        yc = _gla_branch(zc, gla_gate_w[l], gla_gate_b[l], gla_norm_g[l])
        yd = _hgrn2_branch(zd, lower_bounds[l], hgrn_norm_g[l])
        ys = jnp.stack([ya, yb, yc, yd], axis=2)
        gates = jax.nn.sigmoid(zg).reshape(B_, S_, N_BRANCH, D_)
        merged = jnp.sum(jnp.einsum('bsnw,nwd->bsnd', ys, w_branch_out[l]) * gates, axis=2)
        x = x + g1 * (merged @ w_o[l])
        h = _rmsnorm(x, norm_mlp_g[l]) * (1.0 + sc2) + sh2
        x = x + g2 * (jnp.square(jax.nn.relu(h @ mlp_w1[l])) @ mlp_w2[l])
    return _rmsnorm(x, final_g)
```

```python
import math
from contextlib import ExitStack
import numpy as np
import concourse.bass as bass
import concourse.mybir as mybir

F32 = mybir.dt.float32
BF16 = mybir.dt.bfloat16
I32 = mybir.dt.int32
U32 = mybir.dt.uint32
AF = mybir.ActivationFunctionType
ALU = mybir.AluOpType
AX = mybir.AxisListType


STRICT = True


class Buf:
    __slots__ = ("lw", "rd")

    def __init__(self):
        self.lw = None
        self.rd = {}


class V:
    __slots__ = ("ap", "bufs")

    def __init__(self, ap, bufs):
        self.ap = ap
        self.bufs = bufs


class Tile:
    def __init__(self, t, nseg=1, seglen=None):
        self.t = t
        self.nseg = nseg
        self.seglen = seglen
        self.bufs = [Buf() for _ in range(nseg)]

    def __getitem__(self, idx):
        return V(self.t[idx], self.bufs)

    def seg(self, s0, s1, idx):
        return V(self.t[idx], self.bufs[s0:s1])

    def fs(self, a, b, pslice=slice(None)):
        s0 = a // self.seglen
        s1 = (b - 1) // self.seglen + 1
        return V(self.t[pslice, a:b], self.bufs[s0:s1])


class Prog:
    ENG = ("pe", "act", "dve", "pool", "sp")

    def __init__(self, nc, n_dsem=24):
        self.nc = nc
        self.es = ExitStack()
        self.eng = {"pe": nc.tensor, "act": nc.scalar, "dve": nc.vector, "pool": nc.gpsimd, "sp": nc.sync}
        self.sem = {}
        self.cnt = {}
        for e in self.ENG:
            self.sem[e] = self.es.enter_context(nc.semaphore("sem_" + e))
            self.cnt[e] = 0
        self.n_dsem = n_dsem
        for i in range(n_dsem):
            k = ("d", i)
            self.sem[k] = self.es.enter_context(nc.semaphore("dsem%d" % i))
            self.cnt[k] = 0
        self.d_next = 0
        self.d_next_sw = 0
        self.seen = {e: {} for e in self.ENG}
        self.nwait = 0
        self.nins = 0
        self.out_tokens = []

    def sbuf(self, name, shape, dtype, nseg=1, seglen=None):
        t = self.es.enter_context(self.nc.sbuf_tensor("s_" + name, list(shape), dtype))
        return Tile(t, nseg, seglen)

    def psum(self, name, shape, dtype=F32):
        t = self.es.enter_context(self.nc.psum_tensor("p_" + name, list(shape), dtype))
        return Tile(t)

    def dram(self, name, shape, dtype, kind="Internal", nseg=1, seglen=None):
        t = self.nc.dram_tensor(name, list(shape), dtype, kind=kind)
        return Tile(t.ap(), nseg, seglen)

    def _wait(self, e, deps):
        need = {}
        for (k, v) in deps:
            if e == "pe" and k == "pe":
                continue
            if need.get(k, 0) < v:
                need[k] = v
        for k, v in need.items():
            if self.seen[e].get(k, 0) < v:
                self.eng[e].wait_ge(self.sem[k], v)
                self.seen[e][k] = v
                self.nwait += 1

    def _deps(self, e, reads, writes):
        deps = []
        for b in reads:
            if b.lw is not None:
                deps.append(b.lw)
        for b in writes:
            if b.lw is not None and (STRICT or b.lw[0] != e):
                deps.append(b.lw)
            for k, v in b.rd.items():
                if STRICT or k != e:
                    deps.append((k, v))
        return deps

    def _commit(self, tok, reads, writes):
        k, v = tok
        for b in reads:
            if b.rd.get(k, 0) < v:
                b.rd[k] = v
        for b in writes:
            b.lw = tok
            b.rd = {}

    def op(self, e, fn, reads, writes):
        rb = [b for v in reads if isinstance(v, V) for b in v.bufs]
        wb = [b for v in writes for b in v.bufs]
        self._wait(e, self._deps(e, rb, wb))
        ins = fn()
        self.cnt[e] += 1
        ins.then_inc(self.sem[e], 1)
        tok = (e, self.cnt[e])
        self._commit(tok, rb, wb)
        self.nins += 1
        return tok

    def dma(self, q, out, in_, is_output=False, **kw):
        rb = list(in_.bufs)
        wb = list(out.bufs)
        half = self.n_dsem // 2
        if q == "pool":
            i = self.d_next_sw
            self.d_next_sw = (self.d_next_sw + 1) % half
        else:
            i = half + self.d_next
            self.d_next = (self.d_next + 1) % (self.n_dsem - half)
        k = ("d", i)
        deps = self._deps(q, rb, wb)
        if self.cnt[k] > 0:
            deps.append((k, self.cnt[k]))
        self._wait(q, deps)
        ins = self.eng[q].dma_start(out=out.ap, in_=in_.ap, **kw)
        self.cnt[k] += 16
        ins.then_inc(self.sem[k], 16)
        tok = (k, self.cnt[k])
        self._commit(tok, rb, wb)
        self.nins += 1
        if is_output:
            self.out_tokens.append(tok)
        return tok

    def finish(self):
        deps = [(k, c) for k, c in self.cnt.items() if c > 0]
        for e in self.ENG:
            self._wait(e, deps)

    def _a(self, x):
        return x.ap if isinstance(x, V) else x

    def mm(self, out, lhsT, rhs, start=True, stop=True, **kw):
        return self.op("pe", lambda: self.nc.tensor.matmul(out.ap, lhsT.ap, rhs.ap, start=start, stop=stop, **kw),
                       [lhsT, rhs], [out])

    def transpose(self, out, in_, ident):
        return self.op("pe", lambda: self.nc.tensor.transpose(out.ap, in_.ap, ident.ap), [in_, ident], [out])

    def act(self, out, in_, func, bias=None, scale=None, accum_out=None, e="act"):
        kw = {}
        if bias is not None:
            kw["bias"] = self._a(bias)
        if scale is not None:
            kw["scale"] = self._a(scale)
        w = [out]
        if accum_out is not None:
            kw["accum_out"] = accum_out.ap
            w.append(accum_out)
        return self.op("act", lambda: self.nc.scalar.activation(out.ap, in_.ap, func, **kw),
                       [in_, bias, scale], w)

    def ts(self, out, in0, s1, s2=None, op0=ALU.mult, op1=None, accum_out=None, e="dve"):
        kw = {}
        if op1 is not None:
            kw["op1"] = op1
        w = [out]
        if accum_out is not None:
            kw["accum_out"] = accum_out.ap
            w.append(accum_out)
        return self.op(e, lambda: self.eng[e].tensor_scalar(out.ap, in0.ap, self._a(s1), self._a(s2), op0, **kw),
                       [in0, s1, s2], w)

    def tt(self, out, in0, in1, op, e="dve"):
        return self.op(e, lambda: self.eng[e].tensor_tensor(out.ap, in0.ap, in1.ap, op), [in0, in1], [out])

    def stt(self, out, in0, scalar, in1, op0, op1, accum_out=None):
        kw = {}
        w = [out]
        if accum_out is not None:
            kw["accum_out"] = accum_out.ap
            w.append(accum_out)
        return self.op("dve", lambda: self.nc.vector.scalar_tensor_tensor(out.ap, in0.ap, self._a(scalar), in1.ap,
                                                                          op0, op1, **kw),
                       [in0, scalar, in1], w)

    def scan(self, out, d0, d1, initial, op0, op1):
        return self.op("dve", lambda: self.nc.vector.tensor_tensor_scan(out.ap, d0.ap, d1.ap, self._a(initial),
                                                                        op0, op1),
                       [d0, d1, initial], [out])

    def copy(self, out, in_, e="dve"):
        if e == "act":
            return self.op("act", lambda: self.nc.scalar.copy(out.ap, in_.ap), [in_], [out])
        return self.op(e, lambda: self.eng[e].tensor_copy(out.ap, in_.ap), [in_], [out])

    def memset(self, out, val, e="dve"):
        return self.op(e, lambda: self.eng[e].memset(out.ap, val), [], [out])

    def recip(self, out, in_):
        return self.op("dve", lambda: self.nc.vector.reciprocal(out.ap, in_.ap), [in_], [out])

    def reduce(self, out, in_, op, axis=AX.X):
        return self.op("dve", lambda: self.nc.vector.tensor_reduce(out.ap, in_.ap, axis, op), [in_], [out])


D = 1024
KC = 8
TB = 512
EPS = 1e-6
NCH = 66
TWO_PI = 2.0 * math.pi
C1 = 6.28125
C2 = TWO_PI - C1


class ParamPack:
    def __init__(self):
        self.cols = []
        self.off = {}
        self.n = 0

    def add(self, name, arr):
        arr = np.ascontiguousarray(arr, dtype=np.float32).reshape(128, -1)
        self.off[name] = (self.n, arr.shape[1])
        self.cols.append(arr)
        self.n += arr.shape[1]

    def array(self):
        return np.concatenate(self.cols, axis=1)


def param_layout(L):
    pp = ParamPack()
    z = lambda n: np.zeros((128, n), np.float32)
    pp.add("cT", z(8)); pp.add("inv", z(1)); pp.add("sgn", z(1)); pp.add("fg", z(8))
    pp.add("lblog", z(2 * L)); pp.add("adab", z(L * 48))
    for l in range(L):
        pp.add(f"nmg{l}", z(8)); pp.add(f"nlg{l}", z(8)); pp.add(f"convb{l}", z(2)); pp.add(f"lng{l}", z(2))
        pp.add(f"lnb{l}", z(2)); pp.add(f"gateb{l}", z(2)); pp.add(f"glag{l}", z(1)); pp.add(f"hgg{l}", z(1))
        pp.add(f"convw{l}", z(62))
    return pp


BIS_RANGE = 8.0
SPLIT_MIN = 1024


def build(nc, S, L, dbg=None, nbis=24, stop=None, act_share=0.55, interleave=True):
    dbg = dbg or set()
    P = Prog(nc)
    NB = S // TB
    NT = S // 128
    pl = param_layout(L)
    NP = pl.n

    xT_d = P.dram("xT", [D, S], F32, "ExternalInput")
    pos_d = P.dram("pos", [1, S], I32, "ExternalInput")
    par_d = P.dram("par", [128, NP], F32, "ExternalInput")
    adaw_d = P.dram("adaw", [L, D, 6 * D], F32, "ExternalInput")
    win_d = P.dram("win", [L, NCH, 128, KC, 128], F32, "ExternalInput")
    wbo_d = P.dram("wbo", [L, 8, 128, 8, 128], F32, "ExternalInput")
    wo_d = P.dram("wo", [L, 8, 128, 8, 128], F32, "ExternalInput")
    w1_d = P.dram("w1", [L, 32, 128, 8, 128], F32, "ExternalInput")
    w2_d = P.dram("w2", [L, 8, 128, 32, 128], F32, "ExternalInput")
    gw_d = P.dram("gw", [L, 16, 256], F32, "ExternalInput")
    out_d = P.dram("outT", [D, S], F32, "ExternalOutput")
    xs_d = [P.dram(f"xs{i}", [D, S], F32, "Internal") for i in range(max(L - 1, 0))]
    ropeC_d = P.dram("ropeC", [128, S], F32, "Internal")
    ropeS_d = P.dram("ropeS", [128, S], F32, "Internal")
    conv_jobs = {l: [] for l in range(L)}

    def mkw(name, src, n, shape):
        dst = nc.dram_tensor(name + "_b", [L, n] + shape, BF16, kind="Internal").ap()
        tiles = []
        for l in range(L):
            row = []
            for i in range(n):
                t = Tile(dst[l, i])
                conv_jobs[l].append((t, src, l, i))
                row.append(t)
            tiles.append(row)
        return tiles

    def convert(l, a=0, b=None):
        for (t, src, ll, i) in conv_jobs[l][a:b]:
            P.dma("pool", t[:], src[ll, i])

    win_b = mkw("win", win_d, NCH, [128, KC, 128])
    wbo_b = mkw("wbo", wbo_d, 8, [128, 8, 128])
    wo_b = mkw("wo", wo_d, 8, [128, 8, 128])
    w1_b = mkw("w1", w1_d, 32, [128, 8, 128])
    w2_b = mkw("w2", w2_d, 8, [128, 32, 128])
    convert(0)
    if stop == "wconv":
        P.finish(); return P, {}
    dbg_d = {}
    DBG_SHAPES = {"hT": [128, KC * TB], "ya": [128, 2 * TB], "yb": [128, 2 * TB], "yc": [128, 2 * TB],
                  "yd": [128, 2 * TB], "x1": [128, KC * TB], "thr": [128, 4], "mod": [128, L * 48],
                  "rope": [128, 2 * TB], "lb": [128, 2 * L]}
    for name in dbg:
        dbg_d[name] = P.dram("dbg_" + name, DBG_SHAPES[name], F32, "ExternalOutput")

    def dump(name, view, shape):
        return name in dbg

    par = P.sbuf("par", [128, NP], F32)
    P.dma("sp", par[:], par_d[:])

    def pc(name, a=0, n=None):
        o, w = pl.off[name]
        if n is None:
            n = w - a
        return par[:, o + a:o + a + n]

    REG_ZERO = nc.gpsimd.to_reg(0.0)
    REG_NEG = nc.gpsimd.to_reg(-1e30)
    ident = P.sbuf("ident", [128, 128], BF16)
    P.memset(ident[:], 1.0)
    P.op("pool", lambda: nc.gpsimd.affine_select(ident.t[:], ident.t[:], [[-1, 128]], ALU.is_equal, REG_ZERO,
                                                 base=0, channel_multiplier=1), [ident[:]], [ident[:]])
    onesm = P.sbuf("onesm", [128, 128], F32)
    P.memset(onesm[:], 1.0 / D)
    ones256 = P.sbuf("ones256", [128, 128], F32)
    P.memset(ones256[:], 1.0 / 256)
    bd64 = P.sbuf("bd64", [128, 128], F32)
    P.memset(bd64[:], 0.0)
    P.memset(bd64[0:64, 0:64], 1.0 / 64)
    P.memset(bd64[64:128, 64:128], 1.0 / 64)
    onesf = P.sbuf("onesf", [128, 64], F32)
    P.memset(onesf[:], 1.0)

    def build_mask(name, C):
        m = P.sbuf(name, [128, 512], BF16)
        P.memset(m[:], 1.0)
        P.op("pool", lambda: nc.gpsimd.affine_select(m.t[:], m.t[:], [[0, 4], [1, 128]], ALU.is_ge, REG_ZERO,
                                                     base=0, channel_multiplier=-1), [m[:]], [m[:]])
        for b in range(128 // C - 1):
            lo, hi = b * C, (b + 1) * C
            P.op("pool", lambda lo=lo, hi=hi: nc.gpsimd.affine_select(
                m.t[lo:hi, :], m.t[lo:hi, :], [[0, 4], [-1, 128]], ALU.is_ge, REG_ZERO, base=hi - 1,
                channel_multiplier=0), [m[:]], [m[:]])
        return m

    M64 = build_mask("M64", 64)
    M32 = build_mask("M32", 32)

    og = [P.psum(f"og{i}", [128, 512], F32) for i in range(2)]
    opo = P.psum("opo", [128, 512], F32)
    ops_ = P.psum("ops", [128, 512], F32)
    bl = [P.psum(f"bl{i}", [128, 512], F32) for i in range(2)]
    bacc = P.psum("bacc", [128, 512], F32)
    bt = P.psum("bt", [128, 1024], BF16)
    psf = [og[0], og[1], opo, ops_, bl[0], bl[1]]
    st = {"f": 0, "b": 0, "w": 0, "q": 0, "og": 0, "bl": 0}

    def ps():
        t = psf[st["f"] % 6]
        st["f"] += 1
        return t

    def pso():
        t = og[st["og"] % 2]
        st["og"] += 1
        return t

    def psb_():
        t = bl[st["bl"] % 2]
        st["bl"] += 1
        return t

    NWB = 4
    wbuf = [P.sbuf(f"wbuf{i}", [128, KC, 128], BF16) for i in range(NWB)]
    dq = ["sp"]

    def wload(src_view):
        t = wbuf[st["w"] % NWB]
        st["w"] += 1
        q = dq[st["q"] % len(dq)]
        st["q"] += 1
        P.dma(q, t[:], src_view)
        return t

    NTMP = 10
    tmpf = [P.sbuf(f"tmpf{i}", [128, 512], F32) for i in range(NTMP)]
    st["t"] = 0

    def tf():
        t = tmpf[st["t"] % NTMP]
        st["t"] += 1
        return t

    silc = P.sbuf("silc", [128, 8], F32)
    P.act(silc[:], pc("cT"), AF.Silu)
    modv = P.sbuf("modv", [128, L * 48], F32)
    xsb = P.sbuf("xsb", [128, KC, TB], F32)
    awt = [V(xsb.t[:, 2 * i:2 * i + 2, :].rearrange("p a (b c) -> p (a b) c", c=128), xsb.bufs) for i in range(2)]
    for l in range(L):
        pm = ps()
        for j in range(48):
            a = awt[j % 2]
            src = V(adaw_d.t[l].rearrange("(kc p) n -> p kc n", p=128)[:, :, j * 128:(j + 1) * 128], adaw_d.bufs)
            P.dma("sp" if j % 2 == 0 else "act", a, src)
            for kc in range(KC):
                P.mm(pm[:, j:j + 1], V(a.ap[:, kc, :], a.bufs), silc[:, kc:kc + 1], start=(kc == 0), stop=(kc == KC - 1))
        P.tt(modv[:, l * 48:(l + 1) * 48], pm[:, 0:48], pc("adab", l * 48, 48), ALU.add)

    def mod(l, part, kc=None):
        o = l * 48 + part * 8
        if kc is None:
            return modv[:, o:o + 8]
        return modv[:, o + kc:o + kc + 1]

    if stop == "ada":
        P.finish(); return P, {}
    acol = P.sbuf("acol", [128, L * 16], F32)
    for l in range(L):
        for i, (gname, part) in enumerate(((f"nmg{l}", 1), (f"nlg{l}", 4))):
            o = l * 16 + i * 8
            P.ts(acol[:, o:o + 8], mod(l, part), 1.0, None, op0=ALU.add)
            P.tt(acol[:, o:o + 8], acol[:, o:o + 8], pc(gname), ALU.mult)

    lbv = P.sbuf("lbv", [128, 2 * L], F32)
    omlv = P.sbuf("omlv", [128, 2 * L], F32)
    lbt = P.sbuf("lbt", [128, 2 * L + 8], F32)
    for c in range(2):
        lg = pc("lblog", c * L, L)
        mx = lbt[:, 2 * L:2 * L + 1]
        P.reduce(mx, lg, ALU.max)
        nmx = lbt[:, 2 * L + 1:2 * L + 2]
        P.ts(nmx, mx, -1.0, None, op0=ALU.mult)
        e = lbt[:, c * L:(c + 1) * L]
        P.act(e, lg, AF.Exp, bias=nmx, scale=1.0)
        sm = lbt[:, 2 * L + 2:2 * L + 3]
        P.reduce(sm, e, ALU.add)
        rs = lbt[:, 2 * L + 3:2 * L + 4]
        P.recip(rs, sm)
        P.ts(e, e, rs, None, op0=ALU.mult)
        cs = lbt[:, 2 * L + 4:2 * L + 5]
        for l in range(L):
            pl_ = lbt[:, c * L + l:c * L + l + 1]
            if l == 0:
                P.copy(cs, pl_)
            else:
                P.tt(cs, cs, pl_, ALU.add)
            P.tt(lbv[:, c * L + l:c * L + l + 1], cs, lbt[:, c * L:c * L + 1], ALU.subtract)
    P.ts(omlv[:], lbv[:], -1.0, 1.0, op0=ALU.mult, op1=ALU.add)

    if stop == "lb":
        P.finish(); return P, {}
    posi = P.sbuf("posi", [128, 512], I32)
    for blk in range(NB):
        sl = slice(blk * TB, (blk + 1) * TB)
        P.dma("sp", posi[:], V(pos_d.t[0:1, sl].to_broadcast([128, TB]), pos_d.bufs))
        ang = tf(); P.copy(ang[:], posi[:])
        P.ts(ang[:], ang[:], pc("inv"), None, op0=ALU.mult)
        kf = tf(); P.ts(kf[:], ang[:], 1.0 / TWO_PI, None, op0=ALU.mult)
        ki = posi
        kint = P_int = None
        P.copy(posi[:], kf[:])
        P.copy(kf[:], posi[:])
        r = tf()
        P.stt(r[:], kf[:], -C1, ang[:], ALU.mult, ALU.add)
        P.stt(r[:], kf[:], -C2, r[:], ALU.mult, ALU.add)
        P.ts(r[:], r[:], math.pi, -math.pi, op0=ALU.min, op1=ALU.max)
        sn = tf(); P.act(sn[:], r[:], AF.Sin)
        P.ts(sn[:], sn[:], pc("sgn"), None, op0=ALU.mult)
        P.dma("sp", ropeS_d[:, sl], sn[:])
        r2 = tf(); P.ts(r2[:], r[:], math.pi / 2, None, op0=ALU.add)
        wr = tf(); P.ts(wr[:], r2[:], math.pi, -TWO_PI, op0=ALU.is_gt, op1=ALU.mult)
        P.tt(r2[:], r2[:], wr[:], ALU.add)
        P.ts(r2[:], r2[:], math.pi, -math.pi, op0=ALU.min, op1=ALU.max)
        cs_ = tf(); P.act(cs_[:], r2[:], AF.Sin)
        P.dma("sp", ropeC_d[:, sl], cs_[:])

    if stop == "rope":
        P.finish(); return P, {}
    hT = P.sbuf("hT", [128, KC, TB], BF16)
    dgr = [P.sbuf(f"dgr{i}", [128, 128], BF16) for i in range(8)]
    gwb = P.sbuf("gwb", [128, 256], BF16)
    ubuf = [P.sbuf(f"ubuf{c}", [128, 30 + TB], BF16) for c in range(2)]
    kT2 = P.sbuf("kT2", [128, S], BF16, nseg=NB, seglen=TB)
    kiT2 = P.sbuf("kiT2", [128, S], BF16, nseg=NB, seglen=TB)
    vaug = P.sbuf("vaug", [128, NT, 65], BF16)
    qT = P.sbuf("qT", [128, 2, TB], BF16)
    qiT = P.sbuf("qiT", [128, 2, TB], BF16)
    wi_tok = P.sbuf("wi_tok", [128, 4, 4], F32)
    ropeC = P.sbuf("ropeC_sb", [128, TB], F32)
    ropeS = P.sbuf("ropeS_sb", [128, TB], F32)
    yT = [P.sbuf(f"yT{n}", [128, 2, TB], BF16) for n in range(4)]
    bigS = max(S, 8192)
    score = P.sbuf("score", [128, bigS], F32)
    cS32 = P.sbuf("cS32", [128, 2, 64], F32); cS16 = P.sbuf("cS16", [128, 2, 128], BF16)
    dS32 = P.sbuf("dS32", [128, 2, 64], F32); dS16 = P.sbuf("dS16", [128, 2, 128], BF16)
    dqT = P.sbuf("dqT", [128, 2, TB], BF16); dkT = P.sbuf("dkT", [128, 2, TB], BF16)
    dvT = P.sbuf("dvT", [128, 2, TB], BF16); dsog = P.sbuf("dsog", [128, 2, TB], BF16)
    dEq = P.sbuf("dEq", [128, 2, TB], F32)
    vk_tok = [P.sbuf(f"vk_tok{i}", [128, 512], BF16) for i in range(2)]
    k32t = [P.sbuf(f"k32t{i}", [128, 1024], BF16) for i in range(2)]
    v32t = [P.sbuf(f"v32t{i}", [128, 1024], BF16) for i in range(2)]
    Tst = P.sbuf("Tst", [128, 128], F32)
    am = [P.sbuf(f"am{i}", [128, 512], BF16) for i in range(2)]
    osb = P.sbuf("osb", [128, 2, TB], F32)
    tmpw = [P.sbuf(f"tmpw{i}", [128, 1024], BF16) for i in range(4)]
    tmpo = [P.sbuf(f"tmpo{i}", [128, 512], BF16) for i in range(2)]
    st["tb"] = 0
    st["to"] = 0

    class _Half:
        def __init__(self, tile, off):
            self.tile, self.off = tile, off

        def __getitem__(self, idx):
            ps_, fs_ = idx
            a = (fs_.start or 0) + self.off
            b = (fs_.stop if fs_.stop is not None else 256) + self.off
            return self.tile[ps_, a:b]

    def tb():
        i = st["tb"] % 8
        st["tb"] += 1
        return _Half(tmpw[i // 2], (i % 2) * 512)

    def tbo():
        t = tmpo[st["to"] % 2]
        st["to"] += 1
        return t

    mrg = P.sbuf("mrg", [128, KC, TB], BF16, nseg=2)
    uTv = score.t[:, 0:8192].bitcast(BF16).rearrange("p (k t) -> p k t", t=TB)
    junkv = mrg.t[:].rearrange("p k t -> p (k t)").bitcast(mybir.dt.uint8)
    junki = mrg.t[:].rearrange("p k t -> p (k t)").bitcast(mybir.dt.int8)
    wbob = [P.sbuf(f"wbob{i}", [128, 8, 128], BF16) for i in range(2)]
    small = P.sbuf("small", [128, 64], F32)
    thrt = [P.sbuf(f"thr{i}", [128, 2], F32) for i in range(2)]
    cntt = P.sbuf("cnt", [128, 2], F32)
    cnta = P.sbuf("cnta", [128, 1], F32)
    jkd = P.sbuf("jkd", [128, 4], mybir.dt.uint8)
    jka = P.sbuf("jka", [128, 4], mybir.dt.int8)
    dgw = P.sbuf("dgw", [128, 4, 128], BF16)
    yb_tok = P.sbuf("yb_tok", [128, 256], BF16)
    negb = P.sbuf("negb", [128, 2], F32)

    vaug_init = [False]

    def rstd_from(src_view, dst=None):
        r_ = dst if dst is not None else tf()
        P.act(r_[:], src_view, AF.Ln, bias=EPSC[:, 0:1], scale=1.0)
        P.act(r_[:], r_[:], AF.Exp, scale=-0.5)
        return r_

    def sigmoid_x(out_tile, in_view):
        P.act(out_tile[:], in_view, AF.Exp, scale=-1.0)
        P.act(out_tile[:], out_tile[:], AF.Ln, bias=ONEC[:, 0:1], scale=1.0)
        P.act(out_tile[:], out_tile[:], AF.Exp, scale=-1.0)

    def silu_x(out_view, in_view, in_is_psum=False, eng="dve"):
        s_ = tf(); sigmoid_x(s_, in_view)
        if in_is_psum and eng != "dve":
            x_ = tf(); P.copy(x_[:], in_view, e="act")
            P.tt(out_view, x_[:], s_[:], ALU.mult, e=eng)
        else:
            P.tt(out_view, in_view, s_[:], ALU.mult, e=eng)

    def rmsnorm(x, acols, bcols_fn, out_bf):
        pm = ps()
        for kc in range(KC):
            sq = tf()
            P.act(sq[:], x[:, kc, :], AF.Square)
            P.mm(pm[:], onesm[:], sq[:], start=(kc == 0), stop=(kc == KC - 1))
        rstd = rstd_from(pm[:])
        for kc in range(KC):
            t = tf()
            P.stt(t[:], x[:, kc, :], acols[:, kc:kc + 1] if not callable(acols) else acols(kc), rstd[:], ALU.mult, ALU.mult)
            if bcols_fn is None:
                P.copy(out_bf[:, kc, :], t[:], e="act")
            else:
                P.act(out_bf[:, kc, :], t[:], AF.Identity, bias=bcols_fn(kc), scale=1.0)

    EPSC = P.sbuf("epsc", [128, 1], F32)
    P.memset(EPSC[:], EPS)
    ONEC = P.sbuf("onec", [128, 1], F32)
    P.memset(ONEC[:], 1.0)

    def V3(tile, ap):
        return V(ap, tile.bufs)

    def run_tasks(gens):
        acc = [0.0] * len(gens)
        alive = [True] * len(gens)
        while any(alive):
            i = min((k for k in range(len(gens)) if alive[k]), key=lambda k: acc[k])
            try:
                c = next(gens[i])
                acc[i] += (c if c else 1.0)
            except StopIteration:
                alive[i] = False

    for l in range(L):
        x_in = xT_d if l == 0 else xs_d[l - 1]
        x_out = out_d if l == L - 1 else xs_d[l]
        last = (l == L - 1)
        P.dma("pool", gwb[0:16, :], gw_d[l])
        P.ts(negb[:], pc(f"gateb{l}"), -1.0, None, op0=ALU.mult)
        for c in range(2):
            P.memset(ubuf[c][:, 0:30], 0.0)
        P.memset(cS32[:], 0.0); P.memset(cS16[:], 0.0); P.memset(dS32[:], 0.0); P.memset(dS16[:], 0.0)
        if not vaug_init[0]:
            P.memset(vaug[:], 1.0)
            vaug_init[0] = True
        a1 = lambda kc, l=l: acol[:, l * 16 + kc:l * 16 + kc + 1]
        a2 = lambda kc, l=l: acol[:, l * 16 + 8 + kc:l * 16 + 8 + kc + 1]

        for blk in range(NB):
            t0 = blk * TB
            tsl = slice(t0, t0 + TB)
            D0 = (l == 0 and blk == 0)
            if l + 1 < L:
                nj = len(conv_jobs[l + 1])
                per = (nj + NB - 1) // NB
                convert(l + 1, blk * per, min(nj, (blk + 1) * per))
            P.dma("sp", xsb[:], V(x_in.t.rearrange("(kc p) t -> p kc t", p=128)[:, :, tsl], x_in.bufs))
            rmsnorm(xsb, a1, lambda kc: mod(l, 0, kc), hT)
            if dump("hT", None, [128, KC * TB]) and D0:
                for kc in range(KC):
                    t = tf(); P.copy(t[:], hT[:, kc, :]); P.dma("sp", dbg_d["hT"][:, kc * TB:(kc + 1) * TB], t[:])

            def proj(c, psfn=ps):
                w = wload(win_b[l][c][:])
                p_ = psfn()
                for kc in range(KC):
                    P.mm(p_[:], w[:, kc, :], hT[:, kc, :], start=(kc == 0), stop=(kc == KC - 1))
                return p_

            P.dma("sp", ropeC[:], ropeC_d[:, tsl])
            P.dma("sp", ropeS[:], ropeS_d[:, tsl])

            def rope_chunk(cm, cw, outv):
                pz = proj(cm); pw = proj(cw)
                t1 = tf(); P.tt(t1[:], pw[:], ropeS[:], ALU.mult)
                t2 = tf(); P.tt(t2[:], pz[:], ropeC[:], ALU.mult)
                P.tt(outv, t1[:], t2[:], ALU.add, e="pool")

            rope_chunk(4, 6, qT[:, 0, :]); rope_chunk(5, 7, qT[:, 1, :])
            rope_chunk(8, 9, kT2.fs(t0, t0 + TB))
            rope_chunk(11, 13, qiT[:, 0, :]); rope_chunk(12, 14, qiT[:, 1, :])
            rope_chunk(15, 16, kiT2.fs(t0, t0 + TB))
            pv = proj(10)
            vw = tb(); P.copy(vw[:, 0:512], pv[:], e="act")
            for tt_ in range(4):
                P.transpose(bt[:, tt_ * 128:(tt_ + 1) * 128], vw[:, tt_ * 128:(tt_ + 1) * 128], ident[:])
            for tt_ in range(4):
                P.copy(vaug[:, blk * 4 + tt_, 0:64], bt[:, tt_ * 128:tt_ * 128 + 64])
                P.copy(wi_tok[:, tt_, :], bt[:, tt_ * 128 + 64:tt_ * 128 + 68])
            if stop == "Bp":
                P.finish(); return P, {}

            def linattn32(S32, S16):
                for tt_ in range(4):
                    s_ = slice(tt_ * 128, (tt_ + 1) * 128)
                    vt = _Half(vk_tok[tt_ % 2], 0); k32 = k32t[tt_ % 2]; v32 = v32t[tt_ % 2]; amt = am[tt_ % 2]
                    pt = pso()
                    for m in range(2):
                        P.mm(pt[:, m * 128:(m + 1) * 128], dvT[:, m, s_], ident[:])
                    P.copy(vt[:, 0:256], pt[:, 0:256], e="act")
                    for src, dst, eng in ((dkT, k32, "dve"), (dvT, v32, "act")):
                        for hf in range(2):
                            ptk = pso()
                            for s2 in range(2):
                                sub = hf * 2 + s2
                                for m in range(2):
                                    P.mm(ptk[0:32, s2 * 256 + m * 128:s2 * 256 + (m + 1) * 128],
                                         src[:, m, tt_ * 128 + sub * 32:tt_ * 128 + (sub + 1) * 32], ident[:])
                            P.copy(dst[0:32, hf * 512:(hf + 1) * 512], ptk[0:32, :], e=eng)
                    yield 3.0
                    paX = pso(); paY = pso()
                    for h in range(4):
                        m, rr = h // 2, (h % 2) * 64
                        pa_ = paX if rr == 0 else paY
                        P.mm(pa_[:, m * 128:(m + 1) * 128], dkT[rr:rr + 64, m, s_], dqT[rr:rr + 64, m, s_])
                    P.tt(amt[:, 0:256], paX[:, 0:256], M32[:, 0:256], ALU.mult)
                    P.tt(amt[:, 256:512], paY[:, 0:256], M32[:, 0:256], ALU.mult)
                    yield 1.5
                    for sub in range(4):
                        cc = tt_ * 4 + sub
                        cs_ = slice(cc * 32, (cc + 1) * 32)
                        po = opo
                        for m in range(2):
                            for half in range(2):
                                h = 2 * m + half
                                rr = half * 64
                                ac = half * 256 + m * 128 + sub * 32
                                P.mm(po[rr:rr + 64, m * 32:(m + 1) * 32], vt[:, h * 64:(h + 1) * 64],
                                     amt[:, ac:ac + 32], start=True, stop=False)
                            P.mm(po[:, m * 32:(m + 1) * 32], S16[:, m, :], dqT[:, m, cs_], start=False, stop=True)
                        P.copy(V(osb.t[:, :, cs_], osb.bufs),
                               V(po.t[:, 0:64].rearrange("p (m i) -> p m i", m=2), po.bufs), e="act")
                        pS = ops_
                        for h in range(4):
                            m, rr = h // 2, (h % 2) * 64
                            P.mm(pS[rr:rr + 64, m * 64:(m + 1) * 64],
                                 k32[0:32, sub * 256 + h * 64:sub * 256 + (h + 1) * 64],
                                 v32[0:32, sub * 256 + h * 64:sub * 256 + (h + 1) * 64])
                        T_ = V(Tst.t[:, 0:128].rearrange("p (m i) -> p m i", m=2), Tst.bufs)
                        P.tt(T_, V(pS.t[:, 0:128].rearrange("p (m i) -> p m i", m=2), pS.bufs), S32[:], ALU.add)
                        for m in range(2):
                            ecol = dEq[:, m, cc * 32 + 31:cc * 32 + 32]
                            P.act(S32[:, m, :], Tst[:, m * 64:(m + 1) * 64], AF.Identity, scale=ecol)
                            for half in range(2):
                                rs = slice(half * 64, (half + 1) * 64)
                                P.ts(S16[rs, m, half * 64:(half + 1) * 64], Tst[rs, m * 64:(m + 1) * 64],
                                     dEq[rs, m, cc * 32 + 31:cc * 32 + 32], 1.0, op0=ALU.mult, op1=ALU.mult, e="pool")
                        yield 2.5

            def linattn64(S32, S16):
                for tt_ in range(4):
                    s_ = slice(tt_ * 128, (tt_ + 1) * 128)
                    vk = vk_tok[tt_ % 2]; amt = am[tt_ % 2]
                    vt = _Half(vk, 0); kt_ = _Half(vk, 256)
                    pt = pso()
                    for m in range(2):
                        P.mm(pt[:, m * 128:(m + 1) * 128], dvT[:, m, s_], ident[:])
                        P.mm(pt[:, 256 + m * 128:256 + (m + 1) * 128], dkT[:, m, s_], ident[:])
                    P.copy(vk[:], pt[:, 0:512], e="act")
                    paX = pso(); paY = pso()
                    for h in range(4):
                        m, rr = h // 2, (h % 2) * 64
                        pa_ = paX if rr == 0 else paY
                        P.mm(pa_[:, m * 128:(m + 1) * 128], dkT[rr:rr + 64, m, s_], dqT[rr:rr + 64, m, s_])
                    P.tt(amt[:, 0:256], paX[:, 0:256], M64[:, 0:256], ALU.mult)
                    P.tt(amt[:, 256:512], paY[:, 0:256], M64[:, 0:256], ALU.mult)
                    yield 3.0
                    for sub in range(2):
                        cc = tt_ * 2 + sub
                        r0 = sub * 64
                        cs_ = slice(cc * 64, (cc + 1) * 64)
                        po = opo
                        for m in range(2):
                            for half in range(2):
                                h = 2 * m + half
                                rr = half * 64
                                ac = half * 256 + m * 128 + sub * 64
                                P.mm(po[rr:rr + 64, m * 64:(m + 1) * 64], vt[:, h * 64:(h + 1) * 64],
                                     amt[:, ac:ac + 64], start=True, stop=False)
                            P.mm(po[:, m * 64:(m + 1) * 64], S16[:, m, :], dqT[:, m, cs_], start=False, stop=True)
                        P.copy(V(osb.t[:, :, cs_], osb.bufs),
                               V(po.t[:, 0:128].rearrange("p (m i) -> p m i", m=2), po.bufs), e="act")
                        pS = ops_
                        for h in range(4):
                            m, rr = h // 2, (h % 2) * 64
                            P.mm(pS[rr:rr + 64, m * 64:(m + 1) * 64], kt_[r0:r0 + 64, h * 64:(h + 1) * 64],
                                 vt[r0:r0 + 64, h * 64:(h + 1) * 64])
                        T_ = V(Tst.t[:, 0:128].rearrange("p (m i) -> p m i", m=2), Tst.bufs)
                        P.tt(T_, V(pS.t[:, 0:128].rearrange("p (m i) -> p m i", m=2), pS.bufs), S32[:], ALU.add)
                        for m in range(2):
                            ecol = dEq[:, m, cc * 64 + 63:cc * 64 + 64]
                            P.act(S32[:, m, :], Tst[:, m * 64:(m + 1) * 64], AF.Identity, scale=ecol)
                            for half in range(2):
                                rs = slice(half * 64, (half + 1) * 64)
                                P.ts(S16[rs, m, half * 64:(half + 1) * 64], Tst[rs, m * 64:(m + 1) * 64],
                                     dEq[rs, m, cc * 64 + 63:cc * 64 + 64], 1.0, op0=ALU.mult, op1=ALU.mult, e="pool")
                        yield 2.5

            def finalize(gname, yout):
                for m in range(2):
                    sq = tf(); P.act(sq[:], osb[:, m, :], AF.Square)
                    pm_ = pso(); P.mm(pm_[:], bd64[:], sq[:])
                    rs_ = rstd_from(pm_[:])
                    t_ = tf(); P.tt(t_[:], osb[:, m, :], rs_[:], ALU.mult, e="pool")
                    P.stt(yout[:, m, :], t_[:], pc(gname), dsog[:, m, :], ALU.mult, ALU.mult)

            def merge_branch(n_, psfn, in_task):
                for oc in range(8):
                    wb_ = wbob[oc % 2]
                    P.dma("sp", wb_[:, 0:2, :], wbo_b[l][oc][:, n_ * 2:n_ * 2 + 2, :])
                    pg = proj(32 + n_ * 8 + oc, psfn)
                    g_ = tf()
                    if in_task:
                        sigmoid_x(g_, pg[:])
                    else:
                        P.act(g_[:], pg[:], AF.Sigmoid)
                    py = psfn()
                    for kc in range(2):
                        P.mm(py[:], wb_[:, kc, :], yT[n_][:, kc, :], start=(kc == 0), stop=(kc == 1))
                    P.tt(mrg[:, oc, :], py[:], g_[:], ALU.mult)
                    yield 3.0
                for oc2 in range(8):
                    w = wload(wo_b[l][oc2][:])
                    p_ = psfn()
                    for kc in range(KC):
                        P.mm(p_[:], w[:, kc, :], mrg[:, kc, :], start=(kc == 0), stop=(kc == KC - 1))
                    P.stt(xsb[:, oc2, :], p_[:], mod(l, 2, oc2), xsb[:, oc2, :], ALU.mult, ALU.add)
                    yield 2.5

            def gen_O():
                for c in range(2):
                    pv_ = proj(c, pso); pg = proj(2 + c, pso)
                    sg = tf(); sigmoid_x(sg, pg[:])
                    P.tt(ubuf[c][:, 30:30 + TB], pv_[:], sg[:], ALU.mult)
                    yield 4.0
                cb = [tf(), tf()]
                sqc = [tf(), tf()]
                for c in range(2):
                    pcv = pso()
                    for j in range(31):
                        o, _ = pl.off[f"convw{l}"]
                        dgt = dgr[(c * 31 + j) % 8]
                        P.ts(dgt[:], ident[:], par[:, o + c * 31 + j:o + c * 31 + j + 1], 1.0, op0=ALU.mult,
                             op1=ALU.mult, e="pool")
                        P.mm(pcv[:], dgt[:], ubuf[c][:, j:j + TB], start=(j == 0), stop=(j == 30))
                        if j % 8 == 7:
                            yield 2.0
                    P.act(cb[c][:], pcv[:], AF.Identity, bias=pc(f"convb{l}", c, 1), scale=1.0)
                    P.act(sqc[c][:], pcv[:], AF.Square, bias=pc(f"convb{l}", c, 1), scale=1.0)
                    P.copy(ubuf[c][:, 0:30], ubuf[c][:, TB:TB + 30], e="pool")
                    yield 2.0
                pm = pso(); pq = pso()
                for c in range(2):
                    P.mm(pm[:], ones256[:], cb[c][:], start=(c == 0), stop=(c == 1))
                for c in range(2):
                    P.mm(pq[:], ones256[:], sqc[c][:], start=(c == 0), stop=(c == 1))
                mean = tf(); P.copy(mean[:], pm[:], e="act")
                m2 = tf(); P.tt(m2[:], mean[:], mean[:], ALU.mult, e="pool")
                P.tt(m2[:], pq[:], m2[:], ALU.subtract)
                rstd = rstd_from(m2[:], dst=m2)
                yield 4.0
                for c in range(2):
                    d_ = sqc[c]
                    P.tt(d_[:], cb[c][:], mean[:], ALU.subtract, e="pool")
                    P.tt(d_[:], d_[:], rstd[:], ALU.mult)
                    P.act(d_[:], d_[:], AF.Identity, bias=pc(f"lnb{l}", c, 1), scale=pc(f"lng{l}", c, 1))
                    silu_x(yT[0][:, c, :], d_[:], eng="pool")
                yield 3.0
                yield from merge_branch(0, pso, True)
                pgl = proj(23, pso)
                glr = tbo(); P.copy(glr[0:16, :], pgl[0:16, :], e="act")
                for m in range(2):
                    px = pso(); P.mm(px[:], gwb[0:16, m * 128:(m + 1) * 128], glr[0:16, :])
                    e1 = tf(); P.act(e1[:], px[:], AF.Exp, bias=negb[:, m:m + 1], scale=-1.0)
                    l1 = tf(); P.act(l1[:], e1[:], AF.Ln, bias=ONEC[:, 0:1], scale=1.0)
                    lc = tf()
                    for cc in range(8):
                        s_ = slice(cc * 64, (cc + 1) * 64)
                        P.scan(lc[:, s_], onesf[:, 0:64], l1[:, s_], 0.0, ALU.mult, ALU.add)
                    yield 4.0
                    P.act(dEq[:, m, :], lc[:], AF.Exp, scale=-1.0 / 16)
                    ek = tf(); P.act(ek[:], lc[:], AF.Exp, scale=1.0 / 16)
                    pq_ = proj(17 if m == 0 else 64, pso)
                    P.stt(dqT[:, m, :], pq_[:], 32 ** -0.5, dEq[:, m, :], ALU.mult, ALU.mult)
                    pk_ = proj(18 if m == 0 else 65, pso)
                    P.tt(dkT[:, m, :], pk_[:], ek[:], ALU.mult)
                    yield 4.0
                    p_ = proj(19 + m, pso); P.copy(dvT[:, m, :], p_[:], e="act")
                    p_ = proj(21 + m, pso); silu_x(dsog[:, m, :], p_[:], in_is_psum=True, eng="pool")
                    yield 4.0
                yield from linattn64(cS32, cS16)
                finalize(f"glag{l}", yT[2])
                yield 4.0
                yield from merge_branch(2, pso, True)
                for pcx in range(2):
                    pf = proj(24 + pcx, pso)
                    sig = tf(); sigmoid_x(sig, pf[:])
                    sn = tf(); P.ts(sn[:], sig[:], -1.0, 1.0, op0=ALU.mult, op1=ALU.add, e="pool")
                    lbc = lbv[:, pcx * L + l:pcx * L + l + 1]
                    omc = omlv[:, pcx * L + l:pcx * L + l + 1]
                    f_ = tf(); P.ts(f_[:], sig[:], omc, lbc, op0=ALU.mult, op1=ALU.add)
                    lf = tf(); P.act(lf[:], f_[:], AF.Ln)
                    g_ = tf()
                    for cc in range(16):
                        s_ = slice(cc * 32, (cc + 1) * 32)
                        P.scan(g_[:, s_], onesf[:, 0:32], lf[:, s_], 0.0, ALU.mult, ALU.add)
                    yield 4.0
                    P.act(dEq[:, pcx, :], g_[:], AF.Exp)
                    ek = tf(); P.act(ek[:], g_[:], AF.Exp, scale=-1.0)
                    pq_ = proj(26 + pcx, pso)
                    qs = tf(); silu_x(qs[:], pq_[:], in_is_psum=True)
                    P.tt(dqT[:, pcx, :], qs[:], dEq[:, pcx, :], ALU.mult)
                    P.stt(dkT[:, pcx, :], sn[:], omc, ek[:], ALU.mult, ALU.mult)
                    yield 4.0
                    p_ = proj(28 + pcx, pso); P.copy(dvT[:, pcx, :], p_[:], e="act")
                    p_ = proj(30 + pcx, pso); silu_x(dsog[:, pcx, :], p_[:], in_is_psum=True, eng="pool")
                    yield 4.0
                yield from linattn32(dS32, dS16)
                finalize(f"hgg{l}", yT[3])
                yield 4.0
                yield from merge_branch(3, pso, True)

            def gen_B():
                for qt_ in range(4):
                    gq = blk * 4 + qt_
                    nkeys = (gq + 1) * 128
                    qs_ = slice(qt_ * 128, (qt_ + 1) * 128)
                    nkc = (nkeys + 511) // 512
                    for h in range(4):
                        P.ts(dgw[:, h, :], ident[:], wi_tok[:, qt_, h:h + 1], 0.0625, op0=ALU.mult, op1=ALU.mult,
                             e="pool")
                    for kci in range(nkc):
                        n = min(512, nkeys - kci * 512)
                        ks_ = slice(kci * 512, kci * 512 + n)
                        R = []
                        for h in range(4):
                            m, rr = h // 2, (h % 2) * 64
                            p_ = psb_()
                            P.mm(p_[:, 0:n], qiT[rr:rr + 64, m, qs_],
                                 kiT2.fs(kci * 512, kci * 512 + n, slice(rr, rr + 64)))
                            r_ = tb()
                            if h % 2 == 0:
                                P.act(r_[:, 0:n], p_[:, 0:n], AF.Relu)
                            else:
                                P.ts(r_[:, 0:n], p_[:, 0:n], 0.0, None, op0=ALU.max)
                            R.append(r_)
                        for h in range(4):
                            P.mm(bacc[:, 0:n], dgw[:, h, :], R[h][:, 0:n], start=(h == 0), stop=(h == 3))
                        P.copy(score[:, ks_], bacc[:, 0:n])
                        yield 3.5
                    dsl = slice(gq * 128, gq * 128 + 128)
                    P.op("pool", lambda dsl=dsl: nc.gpsimd.affine_select(score.t[:, dsl], score.t[:, dsl],
                                                                          [[-1, 128]], ALU.is_ge, REG_NEG, base=0,
                                                                          channel_multiplier=1),
                         [score[:]], [score[:]])
                    thr = thrt[0]
                    if nkeys <= 256:
                        P.memset(thr[:, 0:1], -1e4)
                    else:
                        na = int(nkeys * act_share) // 128 * 128 if nkeys >= SPLIT_MIN else 0
                        nd = nkeys - na
                        P.memset(thrt[0][:, 0:1], 1.3943e-6)
                        step = BIS_RANGE
                        cur = 0
                        jd = V(jkd.t[:, 0:1].to_broadcast([128, nd]), jkd.bufs)
                        for it in range(nbis):
                            tcur, tnxt = thrt[cur], thrt[1 - cur]
                            P.ts(jd, score[:, 0:nd], tcur[:, 0:1], 0.0, op0=ALU.is_ge, op1=ALU.add,
                                 accum_out=cntt[:, 0:1])
                            if na:
                                ja = V(jka.t[:, 0:1].to_broadcast([128, na]), jka.bufs)
                                P.act(ja, score[:, nd:nkeys], AF.Sign, bias=tcur[:, 0:1], scale=-1.0,
                                      accum_out=cnta[:, 0:1])
                                P.stt(cntt[:, 0:1], cnta[:, 0:1], -0.5, cntt[:, 0:1], ALU.mult, ALU.add)
                                target = 256.0 - na / 2.0
                            else:
                                target = 256.0
                            P.ts(cntt[:, 1:2], cntt[:, 0:1], target, step, op0=ALU.is_ge, op1=ALU.mult)
                            nstep = step / 2 if it < nbis - 1 else step
                            P.stt(tnxt[:, 0:1], cntt[:, 1:2], -nstep, tcur[:, 0:1], ALU.add, ALU.add)
                            cur = 1 - cur
                            step = nstep
                            if it % 2 == 1 or it == nbis - 1:
                                yield 2 * (max(nd * 1.05e-3, na * 0.9e-3) + 1.0)
                        thr = thrt[cur]
                    if dump("thr", None, [128, 4]) and D0:
                        P.dma("sp", dbg_d["thr"][:, qt_:qt_ + 1], thr[:, 0:1])
                    pacc = bacc
                    ntile = nkeys // 128
                    first = [True]
                    npair = 0
                    for kci in range(nkc):
                        n = min(512, nkeys - kci * 512)
                        ks_ = slice(kci * 512, kci * 512 + n)
                        ntc = n // 128
                        mt_ = tmpw[2 + (kci % 2)]
                        P.ts(mt_[:, 0:n], score[:, ks_], thr[:, 0:1], None, op0=ALU.is_ge)
                        for j in range(ntc):
                            P.transpose(bt[:, j * 128:(j + 1) * 128], mt_[:, j * 128:(j + 1) * 128], ident[:])
                        P.copy(mt_[:, 512:512 + n], bt[:, 0:n])
                        for jp in range(0, ntc, 2):
                            nt2 = min(2, ntc - jp)
                            X, Y = bl[0], bl[1]
                            for j2 in range(nt2):
                                kt = kci * 4 + jp + j2
                                for h in range(4):
                                    m, rr = h // 2, (h % 2) * 64
                                    bank = X if rr == 0 else Y
                                    col = (j2 * 2 + m) * 128
                                    P.mm(bank[:, col:col + 128], kT2.fs(kt * 128, (kt + 1) * 128, slice(rr, rr + 64)),
                                         qT[rr:rr + 64, m, qs_])
                            E = tmpw[npair % 2]
                            npair += 1
                            for half, bank in ((0, X), (1, Y)):
                                ev = V(E.t[:, 0:nt2 * 512].rearrange("p (j c q) -> p j c q", c=4, q=128)[:, :, half * 2:half * 2 + 2, :],
                                       E.bufs)
                                bv = V(bank.t[:, 0:nt2 * 256].rearrange("p (j m q) -> p j m q", m=2, q=128), bank.bufs)
                                P.act(ev, bv, AF.Exp, scale=0.125)
                            e4 = V(E.t[:, 0:nt2 * 512].rearrange("p (j c q) -> p j c q", c=4, q=128), E.bufs)
                            mb = V(mt_.t[:, 512 + jp * 128:512 + (jp + nt2) * 128].rearrange("p (j q) -> p j q", q=128)
                                   .unsqueeze(2).to_broadcast([128, nt2, 4, 128]), mt_.bufs)
                            P.tt(e4, e4, mb, ALU.mult)
                            for j2 in range(nt2):
                                kt = kci * 4 + jp + j2
                                for h in range(4):
                                    m, half = h // 2, h % 2
                                    col = j2 * 512 + (half * 2 + m) * 128
                                    P.mm(pacc[:, h * 128:h * 128 + 65], E[:, col:col + 128], vaug[:, kt, :],
                                         start=first[0], stop=(kt == ntile - 1 and h == 3), skip_group_check=True)
                                    first[0] = False
                            yield 2.5
                    rden = small[:, 0:4]
                    P.recip(rden, V(pacc.t[:, :].rearrange("p (h c) -> p h c", h=4)[:, :, 64], pacc.bufs))
                    for h in range(4):
                        P.ts(yb_tok[:, h * 64:(h + 1) * 64], pacc[:, h * 128:h * 128 + 64], small[:, h:h + 1], None,
                             op0=ALU.mult)
                    for m in range(2):
                        P.transpose(bt[:, m * 128:(m + 1) * 128], yb_tok[:, m * 128:(m + 1) * 128], ident[:])
                    P.copy(V(yT[1].t[:, :, qs_], yT[1].bufs),
                           V(bt.t[:, 0:256].rearrange("p (m i) -> p m i", m=2), bt.bufs), e="act")
                    yield 2.0

            if interleave:
                run_tasks([gen_O(), gen_B()])
            else:
                for _ in gen_O():
                    pass
                for _ in gen_B():
                    pass
            if stop == "B":
                P.finish(); return P, {}

            for n_, nm in enumerate(("ya", "yb", "yc", "yd")):
                if dump(nm, None, [128, 2 * TB]) and D0:
                    for m in range(2):
                        t = tf(); P.copy(t[:], yT[n_][:, m, :]); P.dma("sp", dbg_d[nm][:, m * TB:(m + 1) * TB], t[:])

            for _ in merge_branch(1, ps, False):
                pass
            if dump("x1", None, [128, KC * TB]) and D0:
                P.dma("sp", dbg_d["x1"][:], V(xsb.t[:].rearrange("p k t -> p (k t)"), xsb.bufs))

            rmsnorm(xsb, a2, lambda kc: mod(l, 3, kc), hT)
            for fc in range(32):
                w = wload(w1_b[l][fc][:])
                p_ = ps()
                for kc in range(KC):
                    P.mm(p_[:], w[:, kc, :], hT[:, kc, :], start=(kc == 0), stop=(kc == KC - 1))
                r_ = tf(); P.act(r_[:], p_[:], AF.Relu)
                P.tt(V(uTv[:, fc, :], score.bufs), r_[:], r_[:], ALU.mult, e=("dve" if fc % 2 == 0 else "pool"))
            for oc in range(8):
                p_ = ps()
                for g in range(4):
                    w = wload(w2_b[l][oc][:, g * 8:(g + 1) * 8, :])
                    for k8 in range(8):
                        kc = g * 8 + k8
                        P.mm(p_[:], w[:, k8, :], V(uTv[:, kc, :], score.bufs), start=(kc == 0), stop=(kc == 31))
                P.stt(xsb[:, oc, :], p_[:], mod(l, 5, oc), xsb[:, oc, :], ALU.mult, ALU.add)
            if last:
                pm = ps()
                for kc in range(KC):
                    sq = tf(); P.act(sq[:], xsb[:, kc, :], AF.Square)
                    P.mm(pm[:], onesm[:], sq[:], start=(kc == 0), stop=(kc == KC - 1))
                rstd = rstd_from(pm[:])
                for kc in range(KC):
                    ob = tf()
                    P.stt(ob[:], xsb[:, kc, :], pc("fg", kc, 1), rstd[:], ALU.mult, ALU.mult)
                    P.dma("sp", x_out[kc * 128:(kc + 1) * 128, tsl], ob[:], is_output=True)
            else:
                P.dma("sp", V(x_out.t.rearrange("(kc p) t -> p kc t", p=128)[:, :, tsl], x_out.bufs), xsb[:])
    P.finish()
    return P, dbg_d


ROPE_THETA = 500000.0


def win_chunk_cols():
    A0, B0, C0, D0, G0 = 0, 512, 1220, 2004, 3028
    r = lambda a, n: list(range(a, a + n))

    def sw(base, nheads):
        out = []
        for h in range(nheads):
            for d in range(64):
                dd = d + 8 if d < 8 else (d - 8 if d < 16 else d)
                out.append(base + h * 64 + dd)
        return out

    pad = lambda lst: lst + [-1] * (128 - len(lst))
    ch = []
    ch += [r(A0, 128), r(A0 + 128, 128), r(A0 + 256, 128), r(A0 + 384, 128)]
    q0, k0, v0, qi0, ki0, wi0 = B0, B0 + 256, B0 + 320, B0 + 384, B0 + 640, B0 + 704
    qsw = sw(q0, 4)
    ch += [r(q0, 128), r(q0 + 128, 128), qsw[0:128], qsw[128:256]]
    ksw = sw(k0, 1)
    ch += [r(k0, 64) + r(k0, 64), ksw + ksw]
    ch += [pad(r(v0, 64) + r(wi0, 4))]
    qisw = sw(qi0, 4)
    ch += [r(qi0, 128), r(qi0 + 128, 128), qisw[0:128], qisw[128:256]]
    kisw = sw(ki0, 1)
    ch += [r(ki0, 64) + r(ki0, 64), kisw + kisw]
    cq, ck, cv, cog, cg = C0, C0 + 128, C0 + 256, C0 + 512, C0 + 768
    def padh(base, m):
        out = []
        for hh in range(2):
            out += r(base + (2 * m + hh) * 32, 32) + [-1] * 32
        return out
    ch += [padh(cq, 0), padh(ck, 0), r(cv, 128), r(cv + 128, 128), r(cog, 128), r(cog + 128, 128), pad(r(cg, 16))]
    for i in range(8):
        ch.append(r(D0 + i * 128, 128))
    for i in range(32):
        ch.append(r(G0 + i * 128, 128))
    ch += [padh(cq, 1), padh(ck, 1)]
    assert len(ch) == NCH
    return np.array(ch, dtype=np.int64)


def _gpad():
    idx = []
    for m in range(2):
        for hh in range(2):
            idx += list(range((2 * m + hh) * 32, (2 * m + hh) * 32 + 32)) + [-1] * 32
    return np.array(idx)


GPAD = _gpad()


def prep_shared(inp, L):
    f32 = lambda a: np.ascontiguousarray(np.asarray(a), dtype=np.float32)
    cols = win_chunk_cols()
    w_in = f32(inp["w_in"])[:L]
    w_pad = np.concatenate([w_in, np.zeros((L, D, 1), np.float32)], axis=2)
    win = w_pad[:, :, cols.reshape(-1)]
    win = win.reshape(L, KC, 128, NCH, 128).transpose(0, 3, 2, 1, 4)
    wbo = f32(inp["w_branch_out"])[:L]
    wbo = wbo.reshape(L, 4, 2, 128, 8, 128).transpose(0, 4, 3, 1, 2, 5).reshape(L, 8, 128, 8, 128)
    wo = f32(inp["w_o"])[:L].reshape(L, KC, 128, 8, 128).transpose(0, 3, 2, 1, 4)
    w1 = f32(inp["mlp_w1"])[:L].reshape(L, KC, 128, 32, 128).transpose(0, 3, 2, 1, 4)
    w2 = f32(inp["mlp_w2"])[:L].reshape(L, 32, 128, 8, 128).transpose(0, 3, 2, 1, 4)
    sh = {
        "adaw": f32(inp["ada_w"])[:L],
        "win": np.ascontiguousarray(win),
        "wbo": np.ascontiguousarray(wbo),
        "wo": np.ascontiguousarray(wo),
        "w1": np.ascontiguousarray(w1),
        "w2": np.ascontiguousarray(w2),
        "gw": np.ascontiguousarray(np.concatenate([f32(inp["gla_gate_w"])[:L], np.zeros((L, 16, 1), np.float32)], axis=2)[:, :, GPAD]),
    }
    return sh


def prep_params(inp, b, L):
    f32 = lambda a: np.asarray(a, dtype=np.float32)
    pp = ParamPack()
    col8 = lambda v: f32(v).reshape(8, 128).T
    col2 = lambda v: f32(v).reshape(2, 128).T
    pp.add("cT", col8(inp["c"][b]))
    inv = np.zeros((128, 1), np.float32)
    sgn = np.zeros((128, 1), np.float32)
    half = 8
    invf = np.power(np.float32(ROPE_THETA), -np.arange(half, dtype=np.float32) * np.float32(2.0 / 16)).astype(np.float32)
    for p in range(128):
        d = p % 64
        if d < 16:
            inv[p, 0] = invf[d % 8]
            sgn[p, 0] = -1.0 if d < 8 else 1.0
    pp.add("inv", inv); pp.add("sgn", sgn)
    pp.add("fg", col8(inp["final_g"]))
    lbl = f32(inp["hgrn_lb_logits"])[:L]
    pp.add("lblog", lbl.reshape(L, 2, 128).transpose(2, 1, 0).reshape(128, 2 * L))
    adab = f32(inp["ada_b"])[:L]
    pp.add("adab", adab.reshape(L, 48, 128).transpose(2, 0, 1).reshape(128, L * 48))
    for l in range(L):
        pp.add(f"nmg{l}", col8(inp["norm_mix_g"][l])); pp.add(f"nlg{l}", col8(inp["norm_mlp_g"][l]))
        pp.add(f"convb{l}", col2(inp["conv_b"][l])); pp.add(f"lng{l}", col2(inp["conv_ln_g"][l]))
        pp.add(f"lnb{l}", col2(inp["conv_ln_b"][l]))
        gb = np.concatenate([f32(inp["gla_gate_b"][l]), np.zeros(1, np.float32)])[GPAD]
        pp.add(f"gateb{l}", gb.reshape(2, 128).T)
        pp.add(f"glag{l}", np.tile(f32(inp["gla_norm_g"][l]), 2).reshape(128, 1))
        pp.add(f"hgg{l}", np.tile(f32(inp["hgrn_norm_g"][l]), 2).reshape(128, 1))
        cw = f32(inp["conv_w"][l])
        pp.add(f"convw{l}", cw.reshape(31, 2, 128).transpose(2, 1, 0).reshape(128, 62))
    return pp.array()


def prep_core(inp, b, S, L, shared):
    m = dict(shared)
    m["xT"] = np.ascontiguousarray(np.asarray(inp["x"][b], dtype=np.float32)[:S].T)
    m["pos"] = np.ascontiguousarray(np.asarray(inp["positions"][b], dtype=np.int32)[:S].reshape(1, S))
    m["par"] = prep_params(inp, b, L)
    return m


from concourse.bass_utils import run_bass_kernel_spmd

SEQ = 8192
DEPTH = 2
NCORES = 8


def kernel(**inputs):
    nc = bass.Bass("TRN2", target_bir_lowering=False)
    build(nc, SEQ, DEPTH)
    shared = prep_shared(inputs, DEPTH)
    in_maps = [prep_core(inputs, b, SEQ, DEPTH, shared) for b in range(NCORES)]
    res = run_bass_kernel_spmd(nc, in_maps, core_ids=list(range(NCORES)))
    out = np.stack([np.asarray(res.results[b]["outT"]).T for b in range(NCORES)], axis=0)
    return np.ascontiguousarray(out, dtype=np.float32)
```

```python
import math
from contextlib import ExitStack
import numpy as np
import concourse.bass as bass
import concourse.mybir as mybir

F32 = mybir.dt.float32
BF16 = mybir.dt.bfloat16
I32 = mybir.dt.int32
U32 = mybir.dt.uint32
AF = mybir.ActivationFunctionType
ALU = mybir.AluOpType
AX = mybir.AxisListType


STRICT = False


class Buf:
    __slots__ = ("lw", "rd")

    def __init__(self):
        self.lw = None
        self.rd = {}


class V:
    __slots__ = ("ap", "bufs")

    def __init__(self, ap, bufs):
        self.ap = ap
        self.bufs = bufs


class Tile:
    def __init__(self, t, nseg=1, seglen=None):
        self.t = t
        self.nseg = nseg
        self.seglen = seglen
        self.bufs = [Buf() for _ in range(nseg)]

    def __getitem__(self, idx):
        return V(self.t[idx], self.bufs)

    def seg(self, s0, s1, idx):
        return V(self.t[idx], self.bufs[s0:s1])

    def fs(self, a, b, pslice=slice(None)):
        s0 = a // self.seglen
        s1 = (b - 1) // self.seglen + 1
        return V(self.t[pslice, a:b], self.bufs[s0:s1])


class Prog:
    ENG = ("pe", "act", "dve", "pool", "sp")

    def __init__(self, nc, n_dsem=24):
        self.nc = nc
        self.es = ExitStack()
        self.eng = {"pe": nc.tensor, "act": nc.scalar, "dve": nc.vector, "pool": nc.gpsimd, "sp": nc.sync}
        self.sem = {}
        self.cnt = {}
        for e in self.ENG:
            self.sem[e] = self.es.enter_context(nc.semaphore("sem_" + e))
            self.cnt[e] = 0
        self.n_dsem = n_dsem
        for i in range(n_dsem):
            k = ("d", i)
            self.sem[k] = self.es.enter_context(nc.semaphore("dsem%d" % i))
            self.cnt[k] = 0
        self.d_next = 0
        self.d_next_sw = 0
        self.seen = {e: {} for e in self.ENG}
        self.nwait = 0
        self.nins = 0
        self.out_tokens = []

    def sbuf(self, name, shape, dtype, nseg=1, seglen=None):
        t = self.es.enter_context(self.nc.sbuf_tensor("s_" + name, list(shape), dtype))
        return Tile(t, nseg, seglen)

    def psum(self, name, shape, dtype=F32):
        t = self.es.enter_context(self.nc.psum_tensor("p_" + name, list(shape), dtype))
        return Tile(t)

    def dram(self, name, shape, dtype, kind="Internal", nseg=1, seglen=None):
        t = self.nc.dram_tensor(name, list(shape), dtype, kind=kind)
        return Tile(t.ap(), nseg, seglen)

    def _wait(self, e, deps):
        need = {}
        for (k, v) in deps:
            if e == "pe" and k == "pe":
                continue
            if need.get(k, 0) < v:
                need[k] = v
        for k, v in need.items():
            if self.seen[e].get(k, 0) < v:
                self.eng[e].wait_ge(self.sem[k], v)
                self.seen[e][k] = v
                self.nwait += 1

    def _deps(self, e, reads, writes):
        deps = []
        for b in reads:
            if b.lw is not None:
                deps.append(b.lw)
        for b in writes:
            if b.lw is not None and (STRICT or b.lw[0] != e):
                deps.append(b.lw)
            for k, v in b.rd.items():
                if STRICT or k != e:
                    deps.append((k, v))
        return deps

    def _commit(self, tok, reads, writes):
        k, v = tok
        for b in reads:
            if b.rd.get(k, 0) < v:
                b.rd[k] = v
        for b in writes:
            b.lw = tok
            b.rd = {}

    def op(self, e, fn, reads, writes):
        rb = [b for v in reads if isinstance(v, V) for b in v.bufs]
        wb = [b for v in writes for b in v.bufs]
        self._wait(e, self._deps(e, rb, wb))
        ins = fn()
        self.cnt[e] += 1
        ins.then_inc(self.sem[e], 1)
        tok = (e, self.cnt[e])
        self._commit(tok, rb, wb)
        self.nins += 1
        return tok

    def dma(self, q, out, in_, is_output=False, **kw):
        rb = list(in_.bufs)
        wb = list(out.bufs)
        half = self.n_dsem // 2
        if q == "pool":
            i = self.d_next_sw
            self.d_next_sw = (self.d_next_sw + 1) % half
        else:
            i = half + self.d_next
            self.d_next = (self.d_next + 1) % (self.n_dsem - half)
        k = ("d", i)
        deps = self._deps(q, rb, wb)
        if self.cnt[k] > 0:
            deps.append((k, self.cnt[k]))
        self._wait(q, deps)
        ins = self.eng[q].dma_start(out=out.ap, in_=in_.ap, **kw)
        self.cnt[k] += 16
        ins.then_inc(self.sem[k], 16)
        tok = (k, self.cnt[k])
        self._commit(tok, rb, wb)
        self.nins += 1
        if is_output:
            self.out_tokens.append(tok)
        return tok

    def finish(self):
        deps = [(k, c) for k, c in self.cnt.items() if c > 0]
        for e in self.ENG:
            self._wait(e, deps)

    def _a(self, x):
        return x.ap if isinstance(x, V) else x

    def mm(self, out, lhsT, rhs, start=True, stop=True, **kw):
        return self.op("pe", lambda: self.nc.tensor.matmul(out.ap, lhsT.ap, rhs.ap, start=start, stop=stop, **kw),
                       [lhsT, rhs], [out])

    def transpose(self, out, in_, ident):
        return self.op("pe", lambda: self.nc.tensor.transpose(out.ap, in_.ap, ident.ap), [in_, ident], [out])

    def act(self, out, in_, func, bias=None, scale=None, accum_out=None, e="act"):
        kw = {}
        if bias is not None:
            kw["bias"] = self._a(bias)
        if scale is not None:
            kw["scale"] = self._a(scale)
        w = [out]
        if accum_out is not None:
            kw["accum_out"] = accum_out.ap
            w.append(accum_out)
        return self.op("act", lambda: self.nc.scalar.activation(out.ap, in_.ap, func, **kw),
                       [in_, bias, scale], w)

    def ts(self, out, in0, s1, s2=None, op0=ALU.mult, op1=None, accum_out=None, e="dve"):
        kw = {}
        if op1 is not None:
            kw["op1"] = op1
        w = [out]
        if accum_out is not None:
            kw["accum_out"] = accum_out.ap
            w.append(accum_out)
        return self.op(e, lambda: self.eng[e].tensor_scalar(out.ap, in0.ap, self._a(s1), self._a(s2), op0, **kw),
                       [in0, s1, s2], w)

    def tt(self, out, in0, in1, op, e="dve"):
        return self.op(e, lambda: self.eng[e].tensor_tensor(out.ap, in0.ap, in1.ap, op), [in0, in1], [out])

    def stt(self, out, in0, scalar, in1, op0, op1, accum_out=None):
        kw = {}
        w = [out]
        if accum_out is not None:
            kw["accum_out"] = accum_out.ap
            w.append(accum_out)
        return self.op("dve", lambda: self.nc.vector.scalar_tensor_tensor(out.ap, in0.ap, self._a(scalar), in1.ap,
                                                                          op0, op1, **kw),
                       [in0, scalar, in1], w)

    def scan(self, out, d0, d1, initial, op0, op1):
        return self.op("dve", lambda: self.nc.vector.tensor_tensor_scan(out.ap, d0.ap, d1.ap, self._a(initial),
                                                                        op0, op1),
                       [d0, d1, initial], [out])

    def copy(self, out, in_, e="dve"):
        if e == "act":
            return self.op("act", lambda: self.nc.scalar.copy(out.ap, in_.ap), [in_], [out])
        return self.op(e, lambda: self.eng[e].tensor_copy(out.ap, in_.ap), [in_], [out])

    def memset(self, out, val, e="dve"):
        return self.op(e, lambda: self.eng[e].memset(out.ap, val), [], [out])

    def recip(self, out, in_):
        return self.op("dve", lambda: self.nc.vector.reciprocal(out.ap, in_.ap), [in_], [out])

    def reduce(self, out, in_, op, axis=AX.X):
        return self.op("dve", lambda: self.nc.vector.tensor_reduce(out.ap, in_.ap, axis, op), [in_], [out])


D = 1024
KC = 8
TB = 512
EPS = 1e-6
NCH = 66
TWO_PI = 2.0 * math.pi
C1 = 6.28125
C2 = TWO_PI - C1


class ParamPack:
    def __init__(self):
        self.cols = []
        self.off = {}
        self.n = 0

    def add(self, name, arr):
        arr = np.ascontiguousarray(arr, dtype=np.float32).reshape(128, -1)
        self.off[name] = (self.n, arr.shape[1])
        self.cols.append(arr)
        self.n += arr.shape[1]

    def array(self):
        return np.concatenate(self.cols, axis=1)


def param_layout(L):
    pp = ParamPack()
    z = lambda n: np.zeros((128, n), np.float32)
    pp.add("cT", z(8)); pp.add("inv", z(1)); pp.add("sgn", z(1)); pp.add("fg", z(8))
    pp.add("lblog", z(2 * L)); pp.add("adab", z(L * 48))
    for l in range(L):
        pp.add(f"nmg{l}", z(8)); pp.add(f"nlg{l}", z(8)); pp.add(f"convb{l}", z(2)); pp.add(f"lng{l}", z(2))
        pp.add(f"lnb{l}", z(2)); pp.add(f"gateb{l}", z(2)); pp.add(f"glag{l}", z(1)); pp.add(f"hgg{l}", z(1))
        pp.add(f"convw{l}", z(62))
    return pp


def build(nc, S, L, dbg=None, nbis=26, stop=None, act_share=0.5, interleave=True):
    dbg = dbg or set()
    P = Prog(nc)
    NB = S // TB
    NT = S // 128
    pl = param_layout(L)
    NP = pl.n

    xT_d = P.dram("xT", [D, S], F32, "ExternalInput")
    pos_d = P.dram("pos", [1, S], I32, "ExternalInput")
    par_d = P.dram("par", [128, NP], F32, "ExternalInput")
    adaw_d = P.dram("adaw", [L, D, 6 * D], F32, "ExternalInput")
    win_d = P.dram("win", [L, NCH, 128, KC, 128], F32, "ExternalInput")
    wbo_d = P.dram("wbo", [L, 8, 128, 8, 128], F32, "ExternalInput")
    wo_d = P.dram("wo", [L, 8, 128, 8, 128], F32, "ExternalInput")
    w1_d = P.dram("w1", [L, 32, 128, 8, 128], F32, "ExternalInput")
    w2_d = P.dram("w2", [L, 8, 128, 32, 128], F32, "ExternalInput")
    gw_d = P.dram("gw", [L, 16, 256], F32, "ExternalInput")
    out_d = P.dram("outT", [D, S], F32, "ExternalOutput")
    xs_d = [P.dram(f"xs{i}", [D, S], F32, "Internal") for i in range(max(L - 1, 0))]
    ropeC_d = P.dram("ropeC", [128, S], F32, "Internal")
    ropeS_d = P.dram("ropeS", [128, S], F32, "Internal")
    def mkw(name, src, n, shape):
        dst = nc.dram_tensor(name + "_b", [L, n] + shape, BF16, kind="Internal").ap()
        tiles = []
        for l in range(L):
            row = []
            for i in range(n):
                t = Tile(dst[l, i])
                P.dma("pool", t[:], src[l, i])
                row.append(t)
            tiles.append(row)
        return tiles

    win_b = mkw("win", win_d, NCH, [128, KC, 128])
    wbo_b = mkw("wbo", wbo_d, 8, [128, 8, 128])
    wo_b = mkw("wo", wo_d, 8, [128, 8, 128])
    w1_b = mkw("w1", w1_d, 32, [128, 8, 128])
    w2_b = mkw("w2", w2_d, 8, [128, 32, 128])
    if stop == "wconv":
        P.finish(); return P, {}
    dbg_d = {}
    DBG_SHAPES = {"hT": [128, KC * TB], "ya": [128, 2 * TB], "yb": [128, 2 * TB], "yc": [128, 2 * TB],
                  "yd": [128, 2 * TB], "x1": [128, KC * TB], "thr": [128, 4], "mod": [128, L * 48],
                  "rope": [128, 2 * TB], "lb": [128, 2 * L]}
    for name in dbg:
        dbg_d[name] = P.dram("dbg_" + name, DBG_SHAPES[name], F32, "ExternalOutput")

    def dump(name, view, shape):
        return name in dbg

    par = P.sbuf("par", [128, NP], F32)
    P.dma("sp", par[:], par_d[:])

    def pc(name, a=0, n=None):
        o, w = pl.off[name]
        if n is None:
            n = w - a
        return par[:, o + a:o + a + n]

    REG_ZERO = nc.gpsimd.to_reg(0.0)
    REG_NEG = nc.gpsimd.to_reg(-1e30)
    ident = P.sbuf("ident", [128, 128], BF16)
    P.memset(ident[:], 1.0)
    P.op("pool", lambda: nc.gpsimd.affine_select(ident.t[:], ident.t[:], [[-1, 128]], ALU.is_equal, REG_ZERO,
                                                 base=0, channel_multiplier=1), [ident[:]], [ident[:]])
    onesm = P.sbuf("onesm", [128, 128], F32)
    P.memset(onesm[:], 1.0 / D)
    ones256 = P.sbuf("ones256", [128, 128], F32)
    P.memset(ones256[:], 1.0 / 256)
    bd64 = P.sbuf("bd64", [128, 128], F32)
    P.memset(bd64[:], 0.0)
    P.memset(bd64[0:64, 0:64], 1.0 / 64)
    P.memset(bd64[64:128, 64:128], 1.0 / 64)
    onesf = P.sbuf("onesf", [128, 64], F32)
    P.memset(onesf[:], 1.0)

    def build_mask(name, C):
        m = P.sbuf(name, [128, 512], BF16)
        P.memset(m[:], 1.0)
        P.op("pool", lambda: nc.gpsimd.affine_select(m.t[:], m.t[:], [[0, 4], [1, 128]], ALU.is_ge, REG_ZERO,
                                                     base=0, channel_multiplier=-1), [m[:]], [m[:]])
        for b in range(128 // C - 1):
            lo, hi = b * C, (b + 1) * C
            P.op("pool", lambda lo=lo, hi=hi: nc.gpsimd.affine_select(
                m.t[lo:hi, :], m.t[lo:hi, :], [[0, 4], [-1, 128]], ALU.is_ge, REG_ZERO, base=hi - 1,
                channel_multiplier=0), [m[:]], [m[:]])
        return m

    M64 = build_mask("M64", 64)
    M32 = build_mask("M32", 32)

    og = [P.psum(f"og{i}", [128, 512], F32) for i in range(2)]
    opo = P.psum("opo", [128, 512], F32)
    ops_ = P.psum("ops", [128, 512], F32)
    bl = [P.psum(f"bl{i}", [128, 512], F32) for i in range(2)]
    bacc = P.psum("bacc", [128, 512], F32)
    bt = P.psum("bt", [128, 1024], BF16)
    psf = [og[0], og[1], opo, ops_, bl[0], bl[1]]
    st = {"f": 0, "b": 0, "w": 0, "q": 0, "og": 0, "bl": 0}

    def ps():
        t = psf[st["f"] % 6]
        st["f"] += 1
        return t

    def pso():
        t = og[st["og"] % 2]
        st["og"] += 1
        return t

    def psb_():
        t = bl[st["bl"] % 2]
        st["bl"] += 1
        return t

    NWB = 4
    wbuf = [P.sbuf(f"wbuf{i}", [128, KC, 128], BF16) for i in range(NWB)]
    dq = ["sp"]

    def wload(src_view):
        t = wbuf[st["w"] % NWB]
        st["w"] += 1
        q = dq[st["q"] % len(dq)]
        st["q"] += 1
        P.dma(q, t[:], src_view)
        return t

    NTMP = 10
    tmpf = [P.sbuf(f"tmpf{i}", [128, 512], F32) for i in range(NTMP)]
    st["t"] = 0

    def tf():
        t = tmpf[st["t"] % NTMP]
        st["t"] += 1
        return t

    silc = P.sbuf("silc", [128, 8], F32)
    P.act(silc[:], pc("cT"), AF.Silu)
    modv = P.sbuf("modv", [128, L * 48], F32)
    xsb = P.sbuf("xsb", [128, KC, TB], F32)
    awt = [V(xsb.t[:, 2 * i:2 * i + 2, :].rearrange("p a (b c) -> p (a b) c", c=128), xsb.bufs) for i in range(2)]
    for l in range(L):
        pm = ps()
        for j in range(48):
            a = awt[j % 2]
            src = V(adaw_d.t[l].rearrange("(kc p) n -> p kc n", p=128)[:, :, j * 128:(j + 1) * 128], adaw_d.bufs)
            P.dma("sp" if j % 2 == 0 else "act", a, src)
            for kc in range(KC):
                P.mm(pm[:, j:j + 1], V(a.ap[:, kc, :], a.bufs), silc[:, kc:kc + 1], start=(kc == 0), stop=(kc == KC - 1))
        P.tt(modv[:, l * 48:(l + 1) * 48], pm[:, 0:48], pc("adab", l * 48, 48), ALU.add)

    def mod(l, part, kc=None):
        o = l * 48 + part * 8
        if kc is None:
            return modv[:, o:o + 8]
        return modv[:, o + kc:o + kc + 1]

    if stop == "ada":
        P.finish(); return P, {}
    acol = P.sbuf("acol", [128, L * 16], F32)
    for l in range(L):
        for i, (gname, part) in enumerate(((f"nmg{l}", 1), (f"nlg{l}", 4))):
            o = l * 16 + i * 8
            P.ts(acol[:, o:o + 8], mod(l, part), 1.0, None, op0=ALU.add)
            P.tt(acol[:, o:o + 8], acol[:, o:o + 8], pc(gname), ALU.mult)

    lbv = P.sbuf("lbv", [128, 2 * L], F32)
    omlv = P.sbuf("omlv", [128, 2 * L], F32)
    lbt = P.sbuf("lbt", [128, 2 * L + 8], F32)
    for c in range(2):
        lg = pc("lblog", c * L, L)
        mx = lbt[:, 2 * L:2 * L + 1]
        P.reduce(mx, lg, ALU.max)
        nmx = lbt[:, 2 * L + 1:2 * L + 2]
        P.ts(nmx, mx, -1.0, None, op0=ALU.mult)
        e = lbt[:, c * L:(c + 1) * L]
        P.act(e, lg, AF.Exp, bias=nmx, scale=1.0)
        sm = lbt[:, 2 * L + 2:2 * L + 3]
        P.reduce(sm, e, ALU.add)
        rs = lbt[:, 2 * L + 3:2 * L + 4]
        P.recip(rs, sm)
        P.ts(e, e, rs, None, op0=ALU.mult)
        cs = lbt[:, 2 * L + 4:2 * L + 5]
        for l in range(L):
            pl_ = lbt[:, c * L + l:c * L + l + 1]
            if l == 0:
                P.copy(cs, pl_)
            else:
                P.tt(cs, cs, pl_, ALU.add)
            P.tt(lbv[:, c * L + l:c * L + l + 1], cs, lbt[:, c * L:c * L + 1], ALU.subtract)
    P.ts(omlv[:], lbv[:], -1.0, 1.0, op0=ALU.mult, op1=ALU.add)

    if stop == "lb":
        P.finish(); return P, {}
    posi = P.sbuf("posi", [128, 512], I32)
    for blk in range(NB):
        sl = slice(blk * TB, (blk + 1) * TB)
        P.dma("sp", posi[:], V(pos_d.t[0:1, sl].to_broadcast([128, TB]), pos_d.bufs))
        ang = tf(); P.copy(ang[:], posi[:])
        P.ts(ang[:], ang[:], pc("inv"), None, op0=ALU.mult)
        kf = tf(); P.ts(kf[:], ang[:], 1.0 / TWO_PI, None, op0=ALU.mult)
        ki = posi
        kint = P_int = None
        P.copy(posi[:], kf[:])
        P.copy(kf[:], posi[:])
        r = tf()
        P.stt(r[:], kf[:], -C1, ang[:], ALU.mult, ALU.add)
        P.stt(r[:], kf[:], -C2, r[:], ALU.mult, ALU.add)
        P.ts(r[:], r[:], math.pi, -math.pi, op0=ALU.min, op1=ALU.max)
        sn = tf(); P.act(sn[:], r[:], AF.Sin)
        P.ts(sn[:], sn[:], pc("sgn"), None, op0=ALU.mult)
        P.dma("sp", ropeS_d[:, sl], sn[:])
        r2 = tf(); P.ts(r2[:], r[:], math.pi / 2, None, op0=ALU.add)
        wr = tf(); P.ts(wr[:], r2[:], math.pi, -TWO_PI, op0=ALU.is_gt, op1=ALU.mult)
        P.tt(r2[:], r2[:], wr[:], ALU.add)
        P.ts(r2[:], r2[:], math.pi, -math.pi, op0=ALU.min, op1=ALU.max)
        cs_ = tf(); P.act(cs_[:], r2[:], AF.Sin)
        P.dma("sp", ropeC_d[:, sl], cs_[:])

    if stop == "rope":
        P.finish(); return P, {}
    hT = P.sbuf("hT", [128, KC, TB], BF16)
    dgr = [P.sbuf(f"dgr{i}", [128, 128], BF16) for i in range(8)]
    gwb = P.sbuf("gwb", [128, 256], BF16)
    ubuf = [P.sbuf(f"ubuf{c}", [128, 30 + TB], BF16) for c in range(2)]
    kT2 = P.sbuf("kT2", [128, S], BF16, nseg=NB, seglen=TB)
    kiT2 = P.sbuf("kiT2", [128, S], BF16, nseg=NB, seglen=TB)
    vaug = P.sbuf("vaug", [128, NT, 65], BF16)
    qT = P.sbuf("qT", [128, 2, TB], BF16)
    qiT = P.sbuf("qiT", [128, 2, TB], BF16)
    wi_tok = P.sbuf("wi_tok", [128, 4, 4], F32)
    ropeC = P.sbuf("ropeC_sb", [128, TB], F32)
    ropeS = P.sbuf("ropeS_sb", [128, TB], F32)
    yT = [P.sbuf(f"yT{n}", [128, 2, TB], BF16) for n in range(4)]
    bigS = max(S, 8192)
    score = P.sbuf("score", [128, bigS], F32)
    cS32 = P.sbuf("cS32", [128, 2, 64], F32); cS16 = P.sbuf("cS16", [128, 2, 128], BF16)
    dS32 = P.sbuf("dS32", [128, 2, 64], F32); dS16 = P.sbuf("dS16", [128, 2, 128], BF16)
    dqT = P.sbuf("dqT", [128, 2, TB], BF16); dkT = P.sbuf("dkT", [128, 2, TB], BF16)
    dvT = P.sbuf("dvT", [128, 2, TB], BF16); dsog = P.sbuf("dsog", [128, 2, TB], BF16)
    dEq = P.sbuf("dEq", [128, 2, TB], F32)
    v_tok = [P.sbuf(f"v_tok{i}", [128, 256], BF16) for i in range(2)]
    k32t = [P.sbuf(f"k32t{i}", [128, 1024], BF16) for i in range(2)]
    v32t = [P.sbuf(f"v32t{i}", [128, 1024], BF16) for i in range(2)]
    Tst = P.sbuf("Tst", [128, 128], F32)
    am = [P.sbuf(f"am{i}", [128, 512], BF16) for i in range(2)]
    osb = P.sbuf("osb", [128, 2, TB], F32)
    tmpw = [P.sbuf(f"tmpw{i}", [128, 1024], BF16) for i in range(4)]
    tmpo = [P.sbuf(f"tmpo{i}", [128, 512], BF16) for i in range(2)]
    st["tb"] = 0
    st["to"] = 0

    class _Half:
        def __init__(self, tile, off):
            self.tile, self.off = tile, off

        def __getitem__(self, idx):
            ps_, fs_ = idx
            a = (fs_.start or 0) + self.off
            b = (fs_.stop if fs_.stop is not None else 512) + self.off
            return self.tile[ps_, a:b]

    def tb():
        i = st["tb"] % 8
        st["tb"] += 1
        return _Half(tmpw[i // 2], (i % 2) * 512)

    def tbo():
        t = tmpo[st["to"] % 2]
        st["to"] += 1
        return t

    mrg = P.sbuf("mrg", [128, KC, TB], BF16, nseg=2)
    uTv = score.t[:, 0:8192].bitcast(BF16).rearrange("p (k t) -> p k t", t=TB)
    junkv = mrg.t[:].rearrange("p k t -> p (k t)").bitcast(mybir.dt.uint8)
    junki = mrg.t[:].rearrange("p k t -> p (k t)").bitcast(mybir.dt.int8)
    wbob = [P.sbuf(f"wbob{i}", [128, 8, 128], BF16) for i in range(2)]
    small = P.sbuf("small", [128, 64], F32)
    thrt = [P.sbuf(f"thr{i}", [128, 2], F32) for i in range(2)]
    cntt = P.sbuf("cnt", [128, 2], F32)
    cnta = P.sbuf("cnta", [128, 1], F32)
    dgw = P.sbuf("dgw", [128, 4, 128], BF16)
    yb_tok = P.sbuf("yb_tok", [128, 256], BF16)
    negb = P.sbuf("negb", [128, 2], F32)

    vaug_init = [False]

    def rstd_from(src_view, dst=None):
        r_ = dst if dst is not None else tf()
        P.act(r_[:], src_view, AF.Ln, bias=EPSC[:, 0:1], scale=1.0)
        P.act(r_[:], r_[:], AF.Exp, scale=-0.5)
        return r_

    def sigmoid_x(out_tile, in_view):
        P.act(out_tile[:], in_view, AF.Exp, scale=-1.0)
        P.act(out_tile[:], out_tile[:], AF.Ln, bias=ONEC[:, 0:1], scale=1.0)
        P.act(out_tile[:], out_tile[:], AF.Exp, scale=-1.0)

    def silu_x(out_view, in_view, in_is_psum=False, eng="dve"):
        s_ = tf(); sigmoid_x(s_, in_view)
        if in_is_psum and eng != "dve":
            x_ = tf(); P.copy(x_[:], in_view, e="act")
            P.tt(out_view, x_[:], s_[:], ALU.mult, e=eng)
        else:
            P.tt(out_view, in_view, s_[:], ALU.mult, e=eng)

    def rmsnorm(x, acols, bcols_fn, out_bf):
        pm = ps()
        for kc in range(KC):
            sq = tf()
            P.act(sq[:], x[:, kc, :], AF.Square)
            P.mm(pm[:], onesm[:], sq[:], start=(kc == 0), stop=(kc == KC - 1))
        rstd = rstd_from(pm[:])
        for kc in range(KC):
            t = tf()
            P.stt(t[:], x[:, kc, :], acols[:, kc:kc + 1] if not callable(acols) else acols(kc), rstd[:], ALU.mult, ALU.mult)
            if bcols_fn is None:
                P.copy(out_bf[:, kc, :], t[:], e="act")
            else:
                P.act(out_bf[:, kc, :], t[:], AF.Identity, bias=bcols_fn(kc), scale=1.0)

    EPSC = P.sbuf("epsc", [128, 1], F32)
    P.memset(EPSC[:], EPS)
    ONEC = P.sbuf("onec", [128, 1], F32)
    P.memset(ONEC[:], 1.0)

    def V3(tile, ap):
        return V(ap, tile.bufs)

    def run_tasks(gens):
        acc = [0.0] * len(gens)
        alive = [True] * len(gens)
        while any(alive):
            i = min((k for k in range(len(gens)) if alive[k]), key=lambda k: acc[k])
            try:
                c = next(gens[i])
                acc[i] += (c if c else 1.0)
            except StopIteration:
                alive[i] = False

    for l in range(L):
        x_in = xT_d if l == 0 else xs_d[l - 1]
        x_out = out_d if l == L - 1 else xs_d[l]
        last = (l == L - 1)
        P.dma("pool", gwb[0:16, :], gw_d[l])
        P.ts(negb[:], pc(f"gateb{l}"), -1.0, None, op0=ALU.mult)
        for c in range(2):
            P.memset(ubuf[c][:, 0:30], 0.0)
        P.memset(cS32[:], 0.0); P.memset(cS16[:], 0.0); P.memset(dS32[:], 0.0); P.memset(dS16[:], 0.0)
        if not vaug_init[0]:
            P.memset(vaug[:], 1.0)
            vaug_init[0] = True
        a1 = lambda kc, l=l: acol[:, l * 16 + kc:l * 16 + kc + 1]
        a2 = lambda kc, l=l: acol[:, l * 16 + 8 + kc:l * 16 + 8 + kc + 1]

        for blk in range(NB):
            t0 = blk * TB
            tsl = slice(t0, t0 + TB)
            D0 = (l == 0 and blk == 0)
            P.dma("sp", xsb[:], V(x_in.t.rearrange("(kc p) t -> p kc t", p=128)[:, :, tsl], x_in.bufs))
            rmsnorm(xsb, a1, lambda kc: mod(l, 0, kc), hT)
            if dump("hT", None, [128, KC * TB]) and D0:
                for kc in range(KC):
                    t = tf(); P.copy(t[:], hT[:, kc, :]); P.dma("sp", dbg_d["hT"][:, kc * TB:(kc + 1) * TB], t[:])

            def proj(c, psfn=ps):
                w = wload(win_b[l][c][:])
                p_ = psfn()
                for kc in range(KC):
                    P.mm(p_[:], w[:, kc, :], hT[:, kc, :], start=(kc == 0), stop=(kc == KC - 1))
                return p_

            P.dma("sp", ropeC[:], ropeC_d[:, tsl])
            P.dma("sp", ropeS[:], ropeS_d[:, tsl])

            def rope_chunk(cm, cw, outv):
                pz = proj(cm); pw = proj(cw)
                t1 = tf(); P.tt(t1[:], pw[:], ropeS[:], ALU.mult)
                t2 = tf(); P.tt(t2[:], pz[:], ropeC[:], ALU.mult)
                P.tt(outv, t1[:], t2[:], ALU.add, e="pool")

            rope_chunk(4, 6, qT[:, 0, :]); rope_chunk(5, 7, qT[:, 1, :])
            rope_chunk(8, 9, kT2.fs(t0, t0 + TB))
            rope_chunk(11, 13, qiT[:, 0, :]); rope_chunk(12, 14, qiT[:, 1, :])
            rope_chunk(15, 16, kiT2.fs(t0, t0 + TB))
            pv = proj(10)
            vw = tb(); P.copy(vw[:, 0:512], pv[:], e="act")
            for tt_ in range(4):
                P.transpose(bt[:, tt_ * 128:(tt_ + 1) * 128], vw[:, tt_ * 128:(tt_ + 1) * 128], ident[:])
            for tt_ in range(4):
                P.copy(vaug[:, blk * 4 + tt_, 0:64], bt[:, tt_ * 128:tt_ * 128 + 64])
                P.copy(wi_tok[:, tt_, :], bt[:, tt_ * 128 + 64:tt_ * 128 + 68])
            if stop == "Bp":
                P.finish(); return P, {}

            def linattn(S32, S16):
                for tt_ in range(4):
                    s_ = slice(tt_ * 128, (tt_ + 1) * 128)
                    vt = v_tok[tt_ % 2]; k32 = k32t[tt_ % 2]; v32 = v32t[tt_ % 2]; amt = am[tt_ % 2]
                    pt = pso()
                    for m in range(2):
                        P.mm(pt[:, m * 128:(m + 1) * 128], dvT[:, m, s_], ident[:])
                    P.copy(vt[:], pt[:, 0:256], e="act")
                    for src, dst, eng in ((dkT, k32, "dve"), (dvT, v32, "act")):
                        for hf in range(2):
                            ptk = pso()
                            for s2 in range(2):
                                sub = hf * 2 + s2
                                for m in range(2):
                                    P.mm(ptk[0:32, s2 * 256 + m * 128:s2 * 256 + (m + 1) * 128],
                                         src[:, m, tt_ * 128 + sub * 32:tt_ * 128 + (sub + 1) * 32], ident[:])
                            P.copy(dst[0:32, hf * 512:(hf + 1) * 512], ptk[0:32, :], e=eng)
                    yield 3.0
                    paX = pso(); paY = pso()
                    for h in range(4):
                        m, rr = h // 2, (h % 2) * 64
                        pa_ = paX if rr == 0 else paY
                        P.mm(pa_[:, m * 128:(m + 1) * 128], dkT[rr:rr + 64, m, s_], dqT[rr:rr + 64, m, s_])
                    P.tt(amt[:, 0:256], paX[:, 0:256], M32[:, 0:256], ALU.mult)
                    P.tt(amt[:, 256:512], paY[:, 0:256], M32[:, 0:256], ALU.mult)
                    yield 1.5
                    for sub in range(4):
                        cc = tt_ * 4 + sub
                        cs_ = slice(cc * 32, (cc + 1) * 32)
                        po = opo
                        for m in range(2):
                            for half in range(2):
                                h = 2 * m + half
                                rr = half * 64
                                ac = half * 256 + m * 128 + sub * 32
                                P.mm(po[rr:rr + 64, m * 32:(m + 1) * 32], vt[:, h * 64:(h + 1) * 64],
                                     amt[:, ac:ac + 32], start=True, stop=False)
                            P.mm(po[:, m * 32:(m + 1) * 32], S16[:, m, :], dqT[:, m, cs_], start=False, stop=True)
                        P.copy(V(osb.t[:, :, cs_], osb.bufs),
                               V(po.t[:, 0:64].rearrange("p (m i) -> p m i", m=2), po.bufs), e="act")
                        pS = ops_
                        for h in range(4):
                            m, rr = h // 2, (h % 2) * 64
                            P.mm(pS[rr:rr + 64, m * 64:(m + 1) * 64],
                                 k32[0:32, sub * 256 + h * 64:sub * 256 + (h + 1) * 64],
                                 v32[0:32, sub * 256 + h * 64:sub * 256 + (h + 1) * 64])
                        T_ = V(Tst.t[:, 0:128].rearrange("p (m i) -> p m i", m=2), Tst.bufs)
                        P.tt(T_, V(pS.t[:, 0:128].rearrange("p (m i) -> p m i", m=2), pS.bufs), S32[:], ALU.add)
                        for m in range(2):
                            ecol = dEq[:, m, cc * 32 + 31:cc * 32 + 32]
                            P.act(S32[:, m, :], Tst[:, m * 64:(m + 1) * 64], AF.Identity, scale=ecol)
                            for half in range(2):
                                rs = slice(half * 64, (half + 1) * 64)
                                P.ts(S16[rs, m, half * 64:(half + 1) * 64], Tst[rs, m * 64:(m + 1) * 64],
                                     dEq[rs, m, cc * 32 + 31:cc * 32 + 32], 1.0, op0=ALU.mult, op1=ALU.mult, e="pool")
                        yield 2.5

            def finalize(gname, yout):
                for m in range(2):
                    sq = tf(); P.act(sq[:], osb[:, m, :], AF.Square)
                    pm_ = pso(); P.mm(pm_[:], bd64[:], sq[:])
                    rs_ = rstd_from(pm_[:])
                    t_ = tf(); P.tt(t_[:], osb[:, m, :], rs_[:], ALU.mult, e="pool")
                    P.stt(yout[:, m, :], t_[:], pc(gname), dsog[:, m, :], ALU.mult, ALU.mult)

            def gen_O():
                for c in range(2):
                    pv_ = proj(c, pso); pg = proj(2 + c, pso)
                    sg = tf(); sigmoid_x(sg, pg[:])
                    P.tt(ubuf[c][:, 30:30 + TB], pv_[:], sg[:], ALU.mult)
                    yield 4.0
                cb = [tf(), tf()]
                sqc = [tf(), tf()]
                for c in range(2):
                    pcv = pso()
                    for j in range(31):
                        o, _ = pl.off[f"convw{l}"]
                        dgt = dgr[(c * 31 + j) % 8]
                        P.ts(dgt[:], ident[:], par[:, o + c * 31 + j:o + c * 31 + j + 1], 1.0, op0=ALU.mult,
                             op1=ALU.mult, e="pool")
                        P.mm(pcv[:], dgt[:], ubuf[c][:, j:j + TB], start=(j == 0), stop=(j == 30))
                        if j % 8 == 7:
                            yield 2.0
                    P.act(cb[c][:], pcv[:], AF.Identity, bias=pc(f"convb{l}", c, 1), scale=1.0)
                    P.act(sqc[c][:], pcv[:], AF.Square, bias=pc(f"convb{l}", c, 1), scale=1.0)
                    P.copy(ubuf[c][:, 0:30], ubuf[c][:, TB:TB + 30], e="pool")
                    yield 2.0
                pm = pso(); pq = pso()
                for c in range(2):
                    P.mm(pm[:], ones256[:], cb[c][:], start=(c == 0), stop=(c == 1))
                for c in range(2):
                    P.mm(pq[:], ones256[:], sqc[c][:], start=(c == 0), stop=(c == 1))
                mean = tf(); P.copy(mean[:], pm[:], e="act")
                m2 = tf(); P.tt(m2[:], mean[:], mean[:], ALU.mult, e="pool")
                P.tt(m2[:], pq[:], m2[:], ALU.subtract)
                rstd = rstd_from(m2[:], dst=m2)
                yield 4.0
                for c in range(2):
                    d_ = sqc[c]
                    P.tt(d_[:], cb[c][:], mean[:], ALU.subtract, e="pool")
                    P.tt(d_[:], d_[:], rstd[:], ALU.mult)
                    P.act(d_[:], d_[:], AF.Identity, bias=pc(f"lnb{l}", c, 1), scale=pc(f"lng{l}", c, 1))
                    silu_x(yT[0][:, c, :], d_[:], eng="pool")
                yield 3.0
                pgl = proj(23, pso)
                glr = tbo(); P.copy(glr[0:16, :], pgl[0:16, :], e="act")
                for m in range(2):
                    px = pso(); P.mm(px[:], gwb[0:16, m * 128:(m + 1) * 128], glr[0:16, :])
                    e1 = tf(); P.act(e1[:], px[:], AF.Exp, bias=negb[:, m:m + 1], scale=-1.0)
                    l1 = tf(); P.act(l1[:], e1[:], AF.Ln, bias=ONEC[:, 0:1], scale=1.0)
                    lc = tf()
                    for cc in range(16):
                        s_ = slice(cc * 32, (cc + 1) * 32)
                        P.scan(lc[:, s_], onesf[:, 0:32], l1[:, s_], 0.0, ALU.mult, ALU.add)
                    yield 4.0
                    P.act(dEq[:, m, :], lc[:], AF.Exp, scale=-1.0 / 16)
                    ek = tf(); P.act(ek[:], lc[:], AF.Exp, scale=1.0 / 16)
                    pq_ = proj(17 if m == 0 else 64, pso)
                    P.stt(dqT[:, m, :], pq_[:], 32 ** -0.5, dEq[:, m, :], ALU.mult, ALU.mult)
                    pk_ = proj(18 if m == 0 else 65, pso)
                    P.tt(dkT[:, m, :], pk_[:], ek[:], ALU.mult)
                    yield 4.0
                    p_ = proj(19 + m, pso); P.copy(dvT[:, m, :], p_[:], e="act")
                    p_ = proj(21 + m, pso); silu_x(dsog[:, m, :], p_[:], in_is_psum=True, eng="pool")
                    yield 4.0
                yield from linattn(cS32, cS16)
                finalize(f"glag{l}", yT[2])
                yield 4.0
                for pcx in range(2):
                    pf = proj(24 + pcx, pso)
                    sig = tf(); sigmoid_x(sig, pf[:])
                    sn = tf(); P.ts(sn[:], sig[:], -1.0, 1.0, op0=ALU.mult, op1=ALU.add, e="pool")
                    lbc = lbv[:, pcx * L + l:pcx * L + l + 1]
                    omc = omlv[:, pcx * L + l:pcx * L + l + 1]
                    f_ = tf(); P.ts(f_[:], sig[:], omc, lbc, op0=ALU.mult, op1=ALU.add)
                    lf = tf(); P.act(lf[:], f_[:], AF.Ln)
                    g_ = tf()
                    for cc in range(16):
                        s_ = slice(cc * 32, (cc + 1) * 32)
                        P.scan(g_[:, s_], onesf[:, 0:32], lf[:, s_], 0.0, ALU.mult, ALU.add)
                    yield 4.0
                    P.act(dEq[:, pcx, :], g_[:], AF.Exp)
                    ek = tf(); P.act(ek[:], g_[:], AF.Exp, scale=-1.0)
                    pq_ = proj(26 + pcx, pso)
                    qs = tf(); silu_x(qs[:], pq_[:], in_is_psum=True)
                    P.tt(dqT[:, pcx, :], qs[:], dEq[:, pcx, :], ALU.mult)
                    P.stt(dkT[:, pcx, :], sn[:], omc, ek[:], ALU.mult, ALU.mult)
                    yield 4.0
                    p_ = proj(28 + pcx, pso); P.copy(dvT[:, pcx, :], p_[:], e="act")
                    p_ = proj(30 + pcx, pso); silu_x(dsog[:, pcx, :], p_[:], in_is_psum=True, eng="pool")
                    yield 4.0
                yield from linattn(dS32, dS16)
                finalize(f"hgg{l}", yT[3])
                yield 4.0

            def gen_B():
                for qt_ in range(4):
                    gq = blk * 4 + qt_
                    nkeys = (gq + 1) * 128
                    qs_ = slice(qt_ * 128, (qt_ + 1) * 128)
                    nkc = (nkeys + 511) // 512
                    for h in range(4):
                        P.ts(dgw[:, h, :], ident[:], wi_tok[:, qt_, h:h + 1], 0.0625, op0=ALU.mult, op1=ALU.mult,
                             e="pool")
                    for kci in range(nkc):
                        n = min(512, nkeys - kci * 512)
                        ks_ = slice(kci * 512, kci * 512 + n)
                        R = []
                        for h in range(4):
                            m, rr = h // 2, (h % 2) * 64
                            p_ = psb_()
                            P.mm(p_[:, 0:n], qiT[rr:rr + 64, m, qs_],
                                 kiT2.fs(kci * 512, kci * 512 + n, slice(rr, rr + 64)))
                            r_ = tb(); P.act(r_[:, 0:n], p_[:, 0:n], AF.Relu)
                            R.append(r_)
                        for h in range(4):
                            P.mm(bacc[:, 0:n], dgw[:, h, :], R[h][:, 0:n], start=(h == 0), stop=(h == 3))
                        P.copy(score[:, ks_], bacc[:, 0:n])
                        yield 3.5
                    dsl = slice(gq * 128, gq * 128 + 128)
                    P.op("pool", lambda dsl=dsl: nc.gpsimd.affine_select(score.t[:, dsl], score.t[:, dsl],
                                                                          [[-1, 128]], ALU.is_ge, REG_NEG, base=0,
                                                                          channel_multiplier=1),
                         [score[:]], [score[:]])
                    thr = thrt[0]
                    if nkeys <= 256:
                        P.memset(thr[:, 0:1], -1e4)
                    else:
                        na = int(nkeys * act_share) // 128 * 128 if nkeys >= 1024 else 0
                        nd = nkeys - na
                        P.memset(thrt[0][:, 0:1], 1.3943e-6)
                        if na:
                            P.memset(thrt[0][:, 1:2], -1.3943e-6)
                        step = 32.0
                        cur = 0
                        jd = V(junkv[:, 0:nd], mrg.bufs[0:1])
                        for it in range(nbis):
                            tcur, tnxt = thrt[cur], thrt[1 - cur]
                            P.ts(jd, score[:, 0:nd], tcur[:, 0:1], 0.0, op0=ALU.is_ge, op1=ALU.add,
                                 accum_out=cntt[:, 0:1])
                            if na:
                                ja = V(junki[:, 8192 - na:8192], mrg.bufs[1:2])
                                P.act(ja, score[:, nd:nkeys], AF.Sign, bias=tcur[:, 1:2], scale=1.0,
                                      accum_out=cnta[:, 0:1])
                                P.stt(cntt[:, 0:1], cnta[:, 0:1], 0.5, cntt[:, 0:1], ALU.mult, ALU.add)
                                target = 256.0 - na / 2.0
                            else:
                                target = 256.0
                            P.ts(cntt[:, 1:2], cntt[:, 0:1], target, step, op0=ALU.is_ge, op1=ALU.mult)
                            nstep = step / 2 if it < nbis - 1 else step
                            P.stt(tnxt[:, 0:1], cntt[:, 1:2], -nstep, tcur[:, 0:1], ALU.add, ALU.add)
                            if na and it < nbis - 1:
                                P.ts(tnxt[:, 1:2], tnxt[:, 0:1], -1.0, None, op0=ALU.mult)
                            cur = 1 - cur
                            step = nstep
                            yield (nd * 1.05e-3 + 0.8)
                        thr = thrt[cur]
                    if dump("thr", None, [128, 4]) and D0:
                        P.dma("sp", dbg_d["thr"][:, qt_:qt_ + 1], thr[:, 0:1])
                    pacc = bacc
                    ntile = nkeys // 128
                    first = [True]
                    npair = 0
                    for kci in range(nkc):
                        n = min(512, nkeys - kci * 512)
                        ks_ = slice(kci * 512, kci * 512 + n)
                        ntc = n // 128
                        mt_ = tmpw[2 + (kci % 2)]
                        P.ts(mt_[:, 0:n], score[:, ks_], thr[:, 0:1], None, op0=ALU.is_ge)
                        for j in range(ntc):
                            P.transpose(bt[:, j * 128:(j + 1) * 128], mt_[:, j * 128:(j + 1) * 128], ident[:])
                        P.copy(mt_[:, 512:512 + n], bt[:, 0:n])
                        for jp in range(0, ntc, 2):
                            nt2 = min(2, ntc - jp)
                            X, Y = bl[0], bl[1]
                            for j2 in range(nt2):
                                kt = kci * 4 + jp + j2
                                for h in range(4):
                                    m, rr = h // 2, (h % 2) * 64
                                    bank = X if rr == 0 else Y
                                    col = (j2 * 2 + m) * 128
                                    P.mm(bank[:, col:col + 128], kT2.fs(kt * 128, (kt + 1) * 128, slice(rr, rr + 64)),
                                         qT[rr:rr + 64, m, qs_])
                            E = tmpw[npair % 2]
                            npair += 1
                            for half, bank in ((0, X), (1, Y)):
                                ev = V(E.t[:, 0:nt2 * 512].rearrange("p (j c q) -> p j c q", c=4, q=128)[:, :, half * 2:half * 2 + 2, :],
                                       E.bufs)
                                bv = V(bank.t[:, 0:nt2 * 256].rearrange("p (j m q) -> p j m q", m=2, q=128), bank.bufs)
                                P.act(ev, bv, AF.Exp, scale=0.125)
                            e4 = V(E.t[:, 0:nt2 * 512].rearrange("p (j c q) -> p j c q", c=4, q=128), E.bufs)
                            mb = V(mt_.t[:, 512 + jp * 128:512 + (jp + nt2) * 128].rearrange("p (j q) -> p j q", q=128)
                                   .unsqueeze(2).to_broadcast([128, nt2, 4, 128]), mt_.bufs)
                            P.tt(e4, e4, mb, ALU.mult)
                            for j2 in range(nt2):
                                kt = kci * 4 + jp + j2
                                for h in range(4):
                                    m, half = h // 2, h % 2
                                    col = j2 * 512 + (half * 2 + m) * 128
                                    P.mm(pacc[:, h * 128:h * 128 + 65], E[:, col:col + 128], vaug[:, kt, :],
                                         start=first[0], stop=(kt == ntile - 1 and h == 3), skip_group_check=True)
                                    first[0] = False
                            yield 2.5
                    rden = small[:, 0:4]
                    P.recip(rden, V(pacc.t[:, :].rearrange("p (h c) -> p h c", h=4)[:, :, 64], pacc.bufs))
                    for h in range(4):
                        P.ts(yb_tok[:, h * 64:(h + 1) * 64], pacc[:, h * 128:h * 128 + 64], small[:, h:h + 1], None,
                             op0=ALU.mult)
                    for m in range(2):
                        P.transpose(bt[:, m * 128:(m + 1) * 128], yb_tok[:, m * 128:(m + 1) * 128], ident[:])
                    P.copy(V(yT[1].t[:, :, qs_], yT[1].bufs),
                           V(bt.t[:, 0:256].rearrange("p (m i) -> p m i", m=2), bt.bufs), e="act")
                    yield 2.0

            if interleave:
                run_tasks([gen_O(), gen_B()])
            else:
                for _ in gen_O():
                    pass
                for _ in gen_B():
                    pass
            if stop == "B":
                P.finish(); return P, {}

            for n_, nm in enumerate(("ya", "yb", "yc", "yd")):
                if dump(nm, None, [128, 2 * TB]) and D0:
                    for m in range(2):
                        t = tf(); P.copy(t[:], yT[n_][:, m, :]); P.dma("sp", dbg_d[nm][:, m * TB:(m + 1) * TB], t[:])

            for oc in range(8):
                wb_ = wbob[oc % 2]
                P.dma("sp", wb_[:], wbo_b[l][oc][:])
                for n_ in range(4):
                    pg = proj(32 + n_ * 8 + oc)
                    g_ = tf(); P.act(g_[:], pg[:], AF.Sigmoid)
                    py = ps()
                    for kc in range(2):
                        P.mm(py[:], wb_[:, n_ * 2 + kc, :], yT[n_][:, kc, :], start=(kc == 0), stop=(kc == 1))
                    if n_ == 0:
                        mo = tf()
                        P.tt(mo[:], py[:], g_[:], ALU.mult)
                    else:
                        t_ = tf(); P.tt(t_[:], py[:], g_[:], ALU.mult)
                        if n_ < 3:
                            P.tt(mo[:], mo[:], t_[:], ALU.add, e="pool")
                        else:
                            P.tt(mrg[:, oc, :], mo[:], t_[:], ALU.add, e="pool")
            for oc in range(8):
                w = wload(wo_b[l][oc][:])
                p_ = ps()
                for kc in range(KC):
                    P.mm(p_[:], w[:, kc, :], mrg[:, kc, :], start=(kc == 0), stop=(kc == KC - 1))
                P.stt(xsb[:, oc, :], p_[:], mod(l, 2, oc), xsb[:, oc, :], ALU.mult, ALU.add)
            if dump("x1", None, [128, KC * TB]) and D0:
                P.dma("sp", dbg_d["x1"][:], V(xsb.t[:].rearrange("p k t -> p (k t)"), xsb.bufs))

            rmsnorm(xsb, a2, lambda kc: mod(l, 3, kc), hT)
            for fc in range(32):
                w = wload(w1_b[l][fc][:])
                p_ = ps()
                for kc in range(KC):
                    P.mm(p_[:], w[:, kc, :], hT[:, kc, :], start=(kc == 0), stop=(kc == KC - 1))
                r_ = tf(); P.act(r_[:], p_[:], AF.Relu)
                P.tt(V(uTv[:, fc, :], score.bufs), r_[:], r_[:], ALU.mult, e=("dve" if fc % 2 == 0 else "pool"))
            for oc in range(8):
                p_ = ps()
                for g in range(4):
                    w = wload(w2_b[l][oc][:, g * 8:(g + 1) * 8, :])
                    for k8 in range(8):
                        kc = g * 8 + k8
                        P.mm(p_[:], w[:, k8, :], V(uTv[:, kc, :], score.bufs), start=(kc == 0), stop=(kc == 31))
                P.stt(xsb[:, oc, :], p_[:], mod(l, 5, oc), xsb[:, oc, :], ALU.mult, ALU.add)
            if last:
                pm = ps()
                for kc in range(KC):
                    sq = tf(); P.act(sq[:], xsb[:, kc, :], AF.Square)
                    P.mm(pm[:], onesm[:], sq[:], start=(kc == 0), stop=(kc == KC - 1))
                rstd = rstd_from(pm[:])
                for kc in range(KC):
                    ob = tf()
                    P.stt(ob[:], xsb[:, kc, :], pc("fg", kc, 1), rstd[:], ALU.mult, ALU.mult)
                    P.dma("sp", x_out[kc * 128:(kc + 1) * 128, tsl], ob[:], is_output=True)
            else:
                P.dma("sp", V(x_out.t.rearrange("(kc p) t -> p kc t", p=128)[:, :, tsl], x_out.bufs), xsb[:])
    P.finish()
    return P, dbg_d


ROPE_THETA = 500000.0


def win_chunk_cols():
    A0, B0, C0, D0, G0 = 0, 512, 1220, 2004, 3028
    r = lambda a, n: list(range(a, a + n))

    def sw(base, nheads):
        out = []
        for h in range(nheads):
            for d in range(64):
                dd = d + 8 if d < 8 else (d - 8 if d < 16 else d)
                out.append(base + h * 64 + dd)
        return out

    pad = lambda lst: lst + [-1] * (128 - len(lst))
    ch = []
    ch += [r(A0, 128), r(A0 + 128, 128), r(A0 + 256, 128), r(A0 + 384, 128)]
    q0, k0, v0, qi0, ki0, wi0 = B0, B0 + 256, B0 + 320, B0 + 384, B0 + 640, B0 + 704
    qsw = sw(q0, 4)
    ch += [r(q0, 128), r(q0 + 128, 128), qsw[0:128], qsw[128:256]]
    ksw = sw(k0, 1)
    ch += [r(k0, 64) + r(k0, 64), ksw + ksw]
    ch += [pad(r(v0, 64) + r(wi0, 4))]
    qisw = sw(qi0, 4)
    ch += [r(qi0, 128), r(qi0 + 128, 128), qisw[0:128], qisw[128:256]]
    kisw = sw(ki0, 1)
    ch += [r(ki0, 64) + r(ki0, 64), kisw + kisw]
    cq, ck, cv, cog, cg = C0, C0 + 128, C0 + 256, C0 + 512, C0 + 768
    def padh(base, m):
        out = []
        for hh in range(2):
            out += r(base + (2 * m + hh) * 32, 32) + [-1] * 32
        return out
    ch += [padh(cq, 0), padh(ck, 0), r(cv, 128), r(cv + 128, 128), r(cog, 128), r(cog + 128, 128), pad(r(cg, 16))]
    for i in range(8):
        ch.append(r(D0 + i * 128, 128))
    for i in range(32):
        ch.append(r(G0 + i * 128, 128))
    ch += [padh(cq, 1), padh(ck, 1)]
    assert len(ch) == NCH
    return np.array(ch, dtype=np.int64)


def _gpad():
    idx = []
    for m in range(2):
        for hh in range(2):
            idx += list(range((2 * m + hh) * 32, (2 * m + hh) * 32 + 32)) + [-1] * 32
    return np.array(idx)


GPAD = _gpad()


def prep_shared(inp, L):
    f32 = lambda a: np.ascontiguousarray(np.asarray(a), dtype=np.float32)
    cols = win_chunk_cols()
    w_in = f32(inp["w_in"])[:L]
    w_pad = np.concatenate([w_in, np.zeros((L, D, 1), np.float32)], axis=2)
    win = w_pad[:, :, cols.reshape(-1)]
    win = win.reshape(L, KC, 128, NCH, 128).transpose(0, 3, 2, 1, 4)
    wbo = f32(inp["w_branch_out"])[:L]
    wbo = wbo.reshape(L, 4, 2, 128, 8, 128).transpose(0, 4, 3, 1, 2, 5).reshape(L, 8, 128, 8, 128)
    wo = f32(inp["w_o"])[:L].reshape(L, KC, 128, 8, 128).transpose(0, 3, 2, 1, 4)
    w1 = f32(inp["mlp_w1"])[:L].reshape(L, KC, 128, 32, 128).transpose(0, 3, 2, 1, 4)
    w2 = f32(inp["mlp_w2"])[:L].reshape(L, 32, 128, 8, 128).transpose(0, 3, 2, 1, 4)
    sh = {
        "adaw": f32(inp["ada_w"])[:L],
        "win": np.ascontiguousarray(win),
        "wbo": np.ascontiguousarray(wbo),
        "wo": np.ascontiguousarray(wo),
        "w1": np.ascontiguousarray(w1),
        "w2": np.ascontiguousarray(w2),
        "gw": np.ascontiguousarray(np.concatenate([f32(inp["gla_gate_w"])[:L], np.zeros((L, 16, 1), np.float32)], axis=2)[:, :, GPAD]),
    }
    return sh


def prep_params(inp, b, L):
    f32 = lambda a: np.asarray(a, dtype=np.float32)
    pp = ParamPack()
    col8 = lambda v: f32(v).reshape(8, 128).T
    col2 = lambda v: f32(v).reshape(2, 128).T
    pp.add("cT", col8(inp["c"][b]))
    inv = np.zeros((128, 1), np.float32)
    sgn = np.zeros((128, 1), np.float32)
    half = 8
    invf = np.power(np.float32(ROPE_THETA), -np.arange(half, dtype=np.float32) * np.float32(2.0 / 16)).astype(np.float32)
    for p in range(128):
        d = p % 64
        if d < 16:
            inv[p, 0] = invf[d % 8]
            sgn[p, 0] = -1.0 if d < 8 else 1.0
    pp.add("inv", inv); pp.add("sgn", sgn)
    pp.add("fg", col8(inp["final_g"]))
    lbl = f32(inp["hgrn_lb_logits"])[:L]
    pp.add("lblog", lbl.reshape(L, 2, 128).transpose(2, 1, 0).reshape(128, 2 * L))
    adab = f32(inp["ada_b"])[:L]
    pp.add("adab", adab.reshape(L, 48, 128).transpose(2, 0, 1).reshape(128, L * 48))
    for l in range(L):
        pp.add(f"nmg{l}", col8(inp["norm_mix_g"][l])); pp.add(f"nlg{l}", col8(inp["norm_mlp_g"][l]))
        pp.add(f"convb{l}", col2(inp["conv_b"][l])); pp.add(f"lng{l}", col2(inp["conv_ln_g"][l]))
        pp.add(f"lnb{l}", col2(inp["conv_ln_b"][l]))
        gb = np.concatenate([f32(inp["gla_gate_b"][l]), np.zeros(1, np.float32)])[GPAD]
        pp.add(f"gateb{l}", gb.reshape(2, 128).T)
        pp.add(f"glag{l}", np.tile(f32(inp["gla_norm_g"][l]), 2).reshape(128, 1))
        pp.add(f"hgg{l}", np.tile(f32(inp["hgrn_norm_g"][l]), 2).reshape(128, 1))
        cw = f32(inp["conv_w"][l])
        pp.add(f"convw{l}", cw.reshape(31, 2, 128).transpose(2, 1, 0).reshape(128, 62))
    return pp.array()


def prep_core(inp, b, S, L, shared):
    m = dict(shared)
    m["xT"] = np.ascontiguousarray(np.asarray(inp["x"][b], dtype=np.float32)[:S].T)
    m["pos"] = np.ascontiguousarray(np.asarray(inp["positions"][b], dtype=np.int32)[:S].reshape(1, S))
    m["par"] = prep_params(inp, b, L)
    return m


from concourse.bass_utils import run_bass_kernel_spmd

SEQ = 8192
DEPTH = 2
NCORES = 8


def kernel(**inputs):
    nc = bass.Bass("TRN2", target_bir_lowering=False)
    build(nc, SEQ, DEPTH)
    shared = prep_shared(inputs, DEPTH)
    in_maps = [prep_core(inputs, b, SEQ, DEPTH, shared) for b in range(NCORES)]
    res = run_bass_kernel_spmd(nc, in_maps, core_ids=list(range(NCORES)))
    out = np.stack([np.asarray(res.results[b]["outT"]).T for b in range(NCORES)], axis=0)
    return np.ascontiguousarray(out, dtype=np.float32)
```
